# Optimizing a Trainium2 kernel written in Bass

```python
import math
import jax, jax.numpy as jnp
from jax import lax
import numpy as np

D_MODEL = 1024
BATCH = 32
SEQ = 2048
DEPTH = 4

CHUNK = 64
MEM_LEN = 256
Q_BLOCK = 128
CONV_WIDTH = 4
ROPE_THETA = 10000.0
NORM_EPS = 1e-6
LN_EPS = 1e-5

GDN_HEADS = 4
GDN_DK = 128
GDN_DV = 128
GDN_CONV_CH = 2 * GDN_HEADS * GDN_DK + GDN_HEADS * GDN_DV

DIFF_HEADS = 4
DIFF_DQK = 64
DIFF_DV = 2 * DIFF_DQK

SSM_HEADS = 8
SSM_HEADDIM = 64
SSM_GROUPS = 2
SSM_STATE = 128
SSM_INNER = SSM_HEADS * SSM_HEADDIM
SSM_CONV_CH = SSM_INNER + 2 * SSM_GROUPS * SSM_STATE

MEM_HEADS = 4
MEM_HEADDIM = 128

BRANCH_WIDTH = 512
N_BRANCH = 4

N_EXPERTS = 32
TOP_K = 4
D_FF = D_MODEL
SWIGLU_LIMIT = 7.0
SWIGLU_ALPHA = 1.702

DEEPNORM_ALPHA = (2.0 * DEPTH) ** 0.25
DEEPNORM_BETA = (8.0 * DEPTH) ** -0.25

IN_SPLITS = (GDN_HEADS * GDN_DK, GDN_HEADS * GDN_DK, GDN_HEADS * GDN_DV, GDN_HEADS * GDN_DV, GDN_HEADS, GDN_HEADS,
             DIFF_HEADS * 2 * DIFF_DQK, DIFF_HEADS * 2 * DIFF_DQK, DIFF_HEADS * DIFF_DV,
             SSM_INNER, SSM_CONV_CH, SSM_HEADS,
             MEM_HEADS * MEM_HEADDIM,
             N_BRANCH * D_MODEL)
D_IN = sum(IN_SPLITS)

kernel_name = 'hybrid_gdn_diff_ssd_mem_moe_deepnorm'


def _split_cols(h):
    points = [int(c) for c in np.cumsum(IN_SPLITS)[:-1]]
    return jnp.split(h, points, axis=-1)


def _rms_norm(x, w):
    xf = x.astype(jnp.float32)
    y = xf * lax.rsqrt(jnp.mean(xf * xf, axis=-1, keepdims=True) + NORM_EPS)
    return (y * w).astype(x.dtype)


def _layer_norm(x, g, b):
    xf = x.astype(jnp.float32)
    mu = jnp.mean(xf, axis=-1, keepdims=True)
    xc = xf - mu
    var = jnp.mean(xc * xc, axis=-1, keepdims=True)
    return (xc * lax.rsqrt(var + LN_EPS) * g + b).astype(x.dtype)


def _l2norm(x):
    xf = x.astype(jnp.float32)
    return xf * lax.rsqrt(jnp.sum(xf * xf, axis=-1, keepdims=True) + NORM_EPS)


def _causal_dwconv(x, w):
    width, ch = w.shape
    return lax.conv_general_dilated(x, w[:, None, :], window_strides=(1,), padding=((width - 1, 0),),
                                    dimension_numbers=('NWC', 'WIO', 'NWC'), feature_group_count=ch)


def _rope(x, pos):
    half = x.shape[-1] // 2
    inv_freq = ROPE_THETA ** (-jnp.arange(half, dtype=jnp.float32) / half)
    ang = pos.astype(jnp.float32)[:, None] * inv_freq[None, :]
    ang = ang.reshape((ang.shape[0],) + (1,) * (x.ndim - 3) + (half,))
    cos = jnp.cos(ang).astype(x.dtype)
    sin = jnp.sin(ang).astype(x.dtype)
    x1, x2 = x[..., :half], x[..., half:]
    return jnp.concatenate([x1 * cos - x2 * sin, x2 * cos + x1 * sin], axis=-1)


def _gdn_chunked(q, k, v, g, beta):
    bsz, seq, nh, dk = q.shape
    dv = v.shape[-1]
    n = seq // CHUNK
    f32 = jnp.float32

    def chunks(t):
        t = t.astype(f32).reshape((bsz, n, CHUNK, nh) + t.shape[3:])
        return jnp.moveaxis(jnp.moveaxis(t, 1, 0), 2, 3)

    qc, kc, vc, bc = chunks(q), chunks(k), chunks(v), chunks(beta)
    gc = jnp.cumsum(chunks(g), axis=-1)
    idx = jnp.arange(CHUNK)
    tri = idx[:, None] >= idx[None, :]
    decay = jnp.exp(jnp.where(tri, gc[..., :, None] - gc[..., None, :], -jnp.inf))
    kb = kc * bc[..., None]
    a_strict = jnp.where(idx[:, None] > idx[None, :],
                         jnp.einsum('nbhik,nbhjk->nbhij', kb, kc) * decay, 0.0)
    rhs = jnp.concatenate([vc * bc[..., None], kb * jnp.exp(gc)[..., None]], axis=-1)
    sol = lax.linalg.triangular_solve(jnp.eye(CHUNK, dtype=f32) + a_strict, rhs,
                                      left_side=True, lower=True, unit_diagonal=True)
    u, w = sol[..., :dv], sol[..., dv:]
    qk = jnp.einsum('nbhik,nbhjk->nbhij', qc, kc) * decay
    q_dec = qc * jnp.exp(gc)[..., None]
    k_dec = kc * jnp.exp(gc[..., -1:] - gc)[..., None]
    g_last = jnp.exp(gc[..., -1])

    def step(state, inp):
        q_d, k_d, u_n, w_n, qk_n, gl = inp
        v_new = u_n - jnp.einsum('bhik,bhkv->bhiv', w_n, state)
        o = jnp.einsum('bhik,bhkv->bhiv', q_d, state) + jnp.einsum('bhij,bhjv->bhiv', qk_n, v_new)
        state = state * gl[..., None, None] + jnp.einsum('bhik,bhiv->bhkv', k_d, v_new)
        return state, o

    s0 = jnp.zeros((bsz, nh, dk, dv), f32)
    _, o = lax.scan(step, s0, (q_dec, k_dec, u, w, qk, g_last))
    o = jnp.moveaxis(jnp.moveaxis(o, 3, 2), 0, 1)
    return o.reshape(bsz, seq, nh, dv).astype(q.dtype)


def _gated_deltanet(q, k, v, z, b, a, conv_w, a_log, dt_bias, norm_w):
    bsz, seq, _ = q.shape
    qkv = jax.nn.silu(_causal_dwconv(jnp.concatenate([q, k, v], axis=-1), conv_w))
    q, k, v = jnp.split(qkv, [GDN_HEADS * GDN_DK, 2 * GDN_HEADS * GDN_DK], axis=-1)
    q = _l2norm(q.reshape(bsz, seq, GDN_HEADS, GDN_DK)) * GDN_DK ** -0.5
    k = _l2norm(k.reshape(bsz, seq, GDN_HEADS, GDN_DK))
    v = v.reshape(bsz, seq, GDN_HEADS, GDN_DV)
    beta = jax.nn.sigmoid(b.astype(jnp.float32))
    g = -jnp.exp(a_log.astype(jnp.float32)) * jax.nn.softplus((a + dt_bias).astype(jnp.float32))
    o = _gdn_chunked(q, k, v, g, beta).astype(v.dtype)
    o = _rms_norm(o, norm_w) * jax.nn.silu(z.reshape(bsz, seq, GDN_HEADS, GDN_DV))
    return o.reshape(bsz, seq, GDN_HEADS * GDN_DV)


def _diff_attention(q, k, v, lam_params, norm_w, lambda_init):
    bsz, seq, _ = q.shape
    pos = jnp.arange(seq)
    q = _rope(q.reshape(bsz, seq, DIFF_HEADS, 2, DIFF_DQK), pos) * DIFF_DQK ** -0.5
    k = _rope(k.reshape(bsz, seq, DIFF_HEADS, 2, DIFF_DQK), pos)
    v = v.reshape(bsz, seq, DIFF_HEADS, DIFF_DV)
    lp = lam_params.astype(jnp.float32)
    lam = jnp.exp(jnp.sum(lp[0] * lp[1])) - jnp.exp(jnp.sum(lp[2] * lp[3])) + lambda_init
    chunk_id = pos // CHUNK
    outs = []
    for blk in range(seq // Q_BLOCK):
        q0 = blk * Q_BLOCK
        kend = q0 + Q_BLOCK
        s = jnp.einsum('bqhmd,bkhmd->bhmqk', q[:, q0:kend], k[:, :kend]).astype(jnp.float32)
        mask = chunk_id[q0:kend, None] >= chunk_id[None, :kend]
        p = jax.nn.softmax(jnp.where(mask, s, -jnp.inf), axis=-1)
        p = p[:, :, 0] - lam * p[:, :, 1]
        outs.append(jnp.einsum('bhqk,bkhd->bqhd', p.astype(v.dtype), v[:, :kend]))
    o = jnp.concatenate(outs, axis=1)
    o = _rms_norm(o, norm_w) * (1.0 - lambda_init)
    return o.reshape(bsz, seq, DIFF_HEADS * DIFF_DV)


def _ssd_chunked(x, a, bm, cm):
    bsz, seq, nh, hp = x.shape
    ng, ns = bm.shape[2], bm.shape[3]
    r = nh // ng
    n = seq // CHUNK
    f32 = jnp.float32
    xc = x.astype(f32).reshape(bsz, n, CHUNK, ng, r, hp).transpose(1, 0, 2, 3, 4, 5)
    bc = bm.astype(f32).reshape(bsz, n, CHUNK, ng, ns).transpose(1, 0, 2, 3, 4)
    cc = cm.astype(f32).reshape(bsz, n, CHUNK, ng, ns).transpose(1, 0, 2, 3, 4)
    ac = a.astype(f32).reshape(bsz, n, CHUNK, ng, r).transpose(1, 0, 3, 4, 2)
    acum = jnp.cumsum(ac, axis=-1)
    idx = jnp.arange(CHUNK)
    tri = idx[:, None] >= idx[None, :]
    seg = jnp.exp(jnp.where(tri, acum[..., :, None] - acum[..., None, :], -jnp.inf))
    cb = jnp.einsum('nbigd,nbjgd->nbgij', cc, bc)
    y_diag = jnp.einsum('nbgrij,nbjgrp->nbigrp', seg * cb[:, :, :, None], xc)
    decay_in = jnp.exp(acum[..., -1:] - acum)
    states = jnp.einsum('nbjgd,nbgrj,nbjgrp->nbgrpd', bc, decay_in, xc)
    chunk_decay = jnp.exp(acum[..., -1])

    def step(h, inp):
        st, cd = inp
        return h * cd[..., None, None] + st, h

    h0 = jnp.zeros((bsz, ng, r, hp, ns), f32)
    _, h_in = lax.scan(step, h0, (states, chunk_decay))
    y_off = jnp.einsum('nbigd,nbgrpd,nbgri->nbigrp', cc, h_in, jnp.exp(acum))
    y = (y_diag + y_off).transpose(1, 0, 2, 3, 4, 5).reshape(bsz, seq, nh, hp)
    return y.astype(x.dtype)


def _mamba2(z, xbc, dt, conv_w, conv_b, a_log, dt_bias, d_skip, norm_w):
    bsz, seq, _ = z.shape
    xbc = jax.nn.silu(_causal_dwconv(xbc, conv_w) + conv_b)
    xs, bm, cm = jnp.split(xbc, [SSM_INNER, SSM_INNER + SSM_GROUPS * SSM_STATE], axis=-1)
    xs = xs.reshape(bsz, seq, SSM_HEADS, SSM_HEADDIM)
    bm = bm.reshape(bsz, seq, SSM_GROUPS, SSM_STATE)
    cm = cm.reshape(bsz, seq, SSM_GROUPS, SSM_STATE)
    dt = jax.nn.softplus((dt + dt_bias).astype(jnp.float32))
    a = -jnp.exp(a_log.astype(jnp.float32)) * dt
    y = _ssd_chunked(xs * dt[..., None].astype(xs.dtype), a, bm, cm) + d_skip[:, None] * xs
    gated = (y.reshape(bsz, seq, SSM_INNER) * jax.nn.silu(z)).reshape(bsz, seq, SSM_GROUPS, SSM_INNER // SSM_GROUPS)
    y = _rms_norm(gated, norm_w.reshape(SSM_GROUPS, SSM_INNER // SSM_GROUPS))
    return y.reshape(bsz, seq, SSM_INNER)


def _memory_attention(q, mem, w_mem):
    bsz, seq, _ = q.shape
    m = mem.shape[1]
    k, v = jnp.split(mem @ w_mem, 2, axis=-1)
    q = q.reshape(bsz, seq, MEM_HEADS, MEM_HEADDIM) * MEM_HEADDIM ** -0.5
    k = k.reshape(bsz, m, MEM_HEADS, MEM_HEADDIM)
    v = v.reshape(bsz, m, MEM_HEADS, MEM_HEADDIM)
    p = jax.nn.softmax(jnp.einsum('bshd,bmhd->bhsm', q, k).astype(jnp.float32), axis=-1)
    o = jnp.einsum('bhsm,bmhd->bshd', p.astype(v.dtype), v)
    return o.reshape(bsz, seq, MEM_HEADS * MEM_HEADDIM)


def _hybrid_mixer(x, mem, w_in, gdn_conv_w, gdn_a_log, gdn_dt_bias, gdn_norm_w, diff_lambda, diff_norm_w,
                  ssm_conv_w, ssm_conv_b, ssm_a_log, ssm_dt_bias, ssm_d, ssm_norm_w, w_mem, w_branch, w_out,
                  lambda_init):
    bsz, seq, _ = x.shape
    (gq, gk, gv, gz, gb, ga, dq, dk, dv, sz, sxbc, sdt, mq, gate_logits) = _split_cols(x @ w_in)
    y_gdn = _gated_deltanet(gq, gk, gv, gz, gb, ga, gdn_conv_w, gdn_a_log, gdn_dt_bias, gdn_norm_w)
    y_diff = _diff_attention(dq, dk, dv, diff_lambda, diff_norm_w, lambda_init)
    y_ssm = _mamba2(sz, sxbc, sdt, ssm_conv_w, ssm_conv_b, ssm_a_log, ssm_dt_bias, ssm_d, ssm_norm_w)
    y_mem = _memory_attention(mq, mem, w_mem)
    gates = jax.nn.sigmoid(gate_logits.reshape(bsz, seq, N_BRANCH, D_MODEL))
    merged = sum(gates[:, :, i] * (y_i @ w_branch[i]) for i, y_i in enumerate((y_gdn, y_diff, y_ssm, y_mem)))
    return merged @ w_out


def _moe(x, router_w, router_b, w_gate_up, b_gate_up, w_down, b_down):
    bsz, seq, d = x.shape
    xt = x.reshape(bsz * seq, d)
    logits = (xt @ router_w + router_b).astype(jnp.float32)
    top_val, top_idx = lax.top_k(logits, TOP_K)
    top_w = jax.nn.softmax(top_val, axis=-1)
    gate = jnp.einsum('tk,tke->te', top_w, jax.nn.one_hot(top_idx, N_EXPERTS, dtype=jnp.float32)).astype(x.dtype)
    y = jnp.zeros_like(xt)
    for e in range(N_EXPERTS):
        gu = xt @ w_gate_up[e] + b_gate_up[e]
        glu, lin = gu[:, 0::2], gu[:, 1::2]
        glu = jnp.minimum(glu, SWIGLU_LIMIT)
        lin = jnp.clip(lin, -SWIGLU_LIMIT, SWIGLU_LIMIT)
        act = (lin + 1.0) * glu * jax.nn.sigmoid(SWIGLU_ALPHA * glu)
        y = y + gate[:, e:e + 1] * (act @ w_down[e] + b_down[e])
    return y.reshape(bsz, seq, d)


def setup_inputs(seed: int = 0) -> dict:
    key = jax.random.key(seed)
    ks = jax.random.split(key, 28)
    f32 = jnp.float32
    L = DEPTH

    def nrm(k, shape, scale):
        return scale * jax.random.normal(k, shape, f32)

    def gain(k, shape):
        return 1.0 + 0.02 * jax.random.normal(k, shape, f32)

    def dt_bias(k, shape):
        dt = jnp.exp(jax.random.uniform(k, shape, f32, math.log(1e-3), math.log(1e-1)))
        return dt + jnp.log(-jnp.expm1(-dt))

    def a_log(k, shape):
        return jnp.log(jax.random.uniform(k, shape, f32, 1.0, 16.0))

    return {
        'x': nrm(ks[0], (BATCH, SEQ, D_MODEL), 1.0),
        'mem': nrm(ks[1], (BATCH, MEM_LEN, D_MODEL), 1.0),
        'w_in': nrm(ks[2], (L, D_MODEL, D_IN), D_MODEL ** -0.5),
        'gdn_conv_w': nrm(ks[3], (L, CONV_WIDTH, GDN_CONV_CH), CONV_WIDTH ** -0.5),
        'gdn_a_log': a_log(ks[4], (L, GDN_HEADS)),
        'gdn_dt_bias': dt_bias(ks[5], (L, GDN_HEADS)),
        'gdn_norm_w': gain(ks[6], (L, GDN_DV)),
        'diff_lambda': nrm(ks[7], (L, 4, DIFF_DQK), 0.1),
        'diff_norm_w': gain(ks[8], (L, DIFF_DV)),
        'ssm_conv_w': nrm(ks[9], (L, CONV_WIDTH, SSM_CONV_CH), CONV_WIDTH ** -0.5),
        'ssm_conv_b': nrm(ks[10], (L, SSM_CONV_CH), 0.02),
        'ssm_a_log': a_log(ks[11], (L, SSM_HEADS)),
        'ssm_dt_bias': dt_bias(ks[12], (L, SSM_HEADS)),
        'ssm_d': gain(ks[13], (L, SSM_HEADS)),
        'ssm_norm_w': gain(ks[14], (L, SSM_INNER)),
        'w_mem': nrm(ks[15], (L, D_MODEL, 2 * MEM_HEADS * MEM_HEADDIM), D_MODEL ** -0.5),
        'w_branch': nrm(ks[16], (L, N_BRANCH, BRANCH_WIDTH, D_MODEL), BRANCH_WIDTH ** -0.5),
        'w_out': nrm(ks[17], (L, D_MODEL, D_MODEL), DEEPNORM_BETA * D_MODEL ** -0.5),
        'ln1_g': gain(ks[18], (L, D_MODEL)),
        'ln1_b': nrm(ks[19], (L, D_MODEL), 0.02),
        'router_w': nrm(ks[20], (L, D_MODEL, N_EXPERTS), D_MODEL ** -0.5),
        'router_b': nrm(ks[21], (L, N_EXPERTS), 0.01),
        'w_gate_up': nrm(ks[22], (L, N_EXPERTS, D_MODEL, 2 * D_FF), D_MODEL ** -0.5),
        'b_gate_up': nrm(ks[23], (L, N_EXPERTS, 2 * D_FF), 0.01),
        'w_down': nrm(ks[24], (L, N_EXPERTS, D_FF, D_MODEL), DEEPNORM_BETA * D_FF ** -0.5),
        'b_down': nrm(ks[25], (L, N_EXPERTS, D_MODEL), 0.01),
        'ln2_g': gain(ks[26], (L, D_MODEL)),
        'ln2_b': nrm(ks[27], (L, D_MODEL), 0.02),
    }


def reference(x, mem, w_in, gdn_conv_w, gdn_a_log, gdn_dt_bias, gdn_norm_w, diff_lambda, diff_norm_w,
              ssm_conv_w, ssm_conv_b, ssm_a_log, ssm_dt_bias, ssm_d, ssm_norm_w, w_mem, w_branch, w_out,
              ln1_g, ln1_b, router_w, router_b, w_gate_up, b_gate_up, w_down, b_down, ln2_g, ln2_b):
    for l in range(DEPTH):
        lambda_init = 0.8 - 0.6 * math.exp(-0.3 * l)
        mix = _hybrid_mixer(x, mem, w_in[l], gdn_conv_w[l], gdn_a_log[l], gdn_dt_bias[l], gdn_norm_w[l],
                            diff_lambda[l], diff_norm_w[l], ssm_conv_w[l], ssm_conv_b[l], ssm_a_log[l],
                            ssm_dt_bias[l], ssm_d[l], ssm_norm_w[l], w_mem[l], w_branch[l], w_out[l], lambda_init)
        x = _layer_norm(DEEPNORM_ALPHA * x + mix, ln1_g[l], ln1_b[l])
        ffn = _moe(x, router_w[l], router_b[l], w_gate_up[l], b_gate_up[l], w_down[l], b_down[l])
        x = _layer_norm(DEEPNORM_ALPHA * x + ffn, ln2_g[l], ln2_b[l])
    return x
```

```python
import math
from contextlib import contextmanager
import numpy as np
import concourse.bass as bass
import concourse.mybir as mybir
from concourse.bass_utils import run_bass_kernel_spmd

F32 = mybir.dt.float32
BF16 = mybir.dt.bfloat16
AF = mybir.ActivationFunctionType
ALU = mybir.AluOpType
AX = mybir.AxisListType

D_MODEL = 1024
BATCH = 32
SEQ = 2048
DEPTH = 4
MEM_LEN = 256
NORM_EPS = 1e-6
LN_EPS = 1e-5
GDN_HEADS = 4
DIFF_HEADS = 4
DIFF_DQK = 64
SSM_HEADS = 8
SSM_HEADDIM = 64
SSM_GROUPS = 2
SSM_STATE = 128
N_EXPERTS = 32
TOP_K = 4
D_FF = 1024
SWIGLU_LIMIT = 7.0
SWIGLU_ALPHA = 1.702
ROPE_THETA = 10000.0
ALPHA = (2.0 * DEPTH) ** 0.25
D_IN = 9744
C_GQ, C_GK, C_GV, C_GZ, C_GB, C_GA = 0, 512, 1024, 1536, 2048, 2052
C_DQ, C_DK, C_DV = 2056, 2568, 3080
C_SZ, C_SXBC, C_SDT = 3592, 4104, 5128
C_MQ = 5136
C_GATE = 5648
NCORES = 8
NEG = -30000.0

SEM_LIMIT = 30000
DMA_POOL = 8


class Sched:
    def __init__(self, nc):
        self.nc = nc
        self.eng = dict(pe=nc.tensor, act=nc.scalar, dve=nc.vector, pool=nc.gpsimd, sp=nc.sync)
        self.sem = {}
        self.cnt = {}
        self.prev = {}
        self.nsem = 0
        self.all_sems = []
        for e in self.eng:
            self._new_sem(e)
        self.waited = {e: {} for e in self.eng}
        self.lastw = {}
        self.readers = {}
        self.dma = {}
        self.ninst = {e: 0 for e in self.eng}

    def _alloc(self, name):
        self.nsem += 1
        h = self.nc.alloc_semaphore(name=f"{name}_{self.nsem}")
        self.all_sems.append(h)
        return h

    def reset_all(self):
        self.finish()
        self.nc.all_engine_barrier()
        self.nc.gpsimd.dma_reset()
        for h in self.all_sems:
            self.nc.gpsimd.sem_clear(h)
        self.nc.all_engine_barrier()
        for e in self.eng:
            self.cnt[e] = 0
        self.prev = {}
        self.waited = {e: {} for e in self.eng}
        self.lastw = {}
        self.readers = {}
        for q, st in self.dma.items():
            st['vals'] = [0] * len(st['vals'])
            st['i'] = 0

    def _new_sem(self, e):
        if e in self.sem and self.cnt[e] > 0:
            self.prev[e] = (self.sem[e], self.cnt[e])
        self.sem[e] = self._alloc("s_" + e)
        self.cnt[e] = 0

    def _wait(self, e, tok):
        sem, val = tok[0], tok[1]
        w = self.waited[e]
        k = id(sem)
        if w.get(k, 0) >= val:
            return
        self.eng[e].wait_ge(sem, val)
        self.ninst[e] += 1
        w[k] = val

    def _need(self, e, tok, acc):
        sem, val = tok[0], tok[1]
        k = id(sem)
        if self.waited[e].get(k, 0) >= val:
            return
        cur = acc.get(k)
        if cur is None or cur[1] < val:
            acc[k] = (sem, val)

    def _deps(self, e, reads, writes):
        acc = {}
        pe = (e == 'pe')
        for k in reads:
            t = self.lastw.get(k)
            if t is not None and not (pe and t[2] == 'pe'):
                self._need(e, t, acc)
            if type(k) is tuple and k[0] == 'ps':
                rd = self.readers.get(k)
                if rd:
                    for t in rd.values():
                        if t[2] != e:
                            self._need(e, t, acc)
        for k in writes:
            t = self.lastw.get(k)
            if t is not None and not (pe and t[2] == 'pe'):
                self._need(e, t, acc)
            rd = self.readers.get(k)
            if rd:
                for t in rd.values():
                    if not (pe and t[2] == 'pe'):
                        self._need(e, t, acc)
        return list(acc.values())

    def _emit_waits(self, e, needs, ins_fn):
        w = self.waited[e]
        for sem, val in needs[:-1]:
            self.eng[e].wait_ge(sem, val)
            self.ninst[e] += 1
            w[id(sem)] = val
        ins = ins_fn()
        if needs:
            sem, val = needs[-1]
            ins._wait_ge(sem, val)
            w[id(sem)] = val
        return ins

    def _commit(self, tok, reads, writes):
        for k in reads:
            self.readers.setdefault(k, {})[id(tok[0])] = tok
        for k in writes:
            self.lastw[k] = tok
            self.readers[k] = {}

    def op(self, e, fn, reads=(), writes=()):
        needs = self._deps(e, reads, writes)
        if self.cnt[e] >= SEM_LIMIT:
            self._new_sem(e)
        ins = self._emit_waits(e, needs, lambda: fn(self.eng[e]))
        self.cnt[e] += 1
        ins.then_inc(self.sem[e], 1)
        self.ninst[e] += 1
        tok = (self.sem[e], self.cnt[e], e)
        self._commit(tok, reads, writes)
        return tok

    def dma_op(self, q, out, in_, reads=(), writes=(), **kw):
        needs = self._deps(q, reads, writes)
        st = self.dma.setdefault(q, dict(sems=[], vals=[], i=0))
        i = st['i'] % DMA_POOL
        if len(st['sems']) <= i:
            st['sems'].append(self._alloc("d_" + q))
            st['vals'].append(0)
        if st['vals'][i] > 0:
            acc = {id(t[0]): t for t in needs}
            self._need(q, (st['sems'][i], st['vals'][i]), acc)
            needs = list(acc.values())
        newsem = st['vals'][i] >= SEM_LIMIT
        ins = self._emit_waits(q, needs, lambda: self.eng[q].dma_start(out=out, in_=in_, **kw))
        if newsem:
            st['sems'][i] = self._alloc("d_" + q)
            st['vals'][i] = 0
        sem = st['sems'][i]
        st['i'] += 1
        st['vals'][i] += 16
        ins.then_inc(sem, 16)
        self.ninst[q] += 1
        tok = (sem, st['vals'][i], 'dma')
        self._commit(tok, reads, writes)
        return tok

    def barrier(self):
        toks = []
        for f in self.eng:
            if self.cnt[f] > 0:
                toks.append((self.sem[f], self.cnt[f]))
            elif f in self.prev:
                toks.append(self.prev[f])
        for q, st in self.dma.items():
            for sem, v in zip(st['sems'], st['vals']):
                if v:
                    toks.append((sem, v))
        for e in self.eng:
            for t in toks:
                self._wait(e, t)

    def finish(self):
        for q, st in self.dma.items():
            for sem, v in zip(st['sems'], st['vals']):
                if v:
                    self._wait(q, (sem, v))
        self.barrier()


def host_consts():
    c = {}
    i = np.arange(128)
    c['ident'] = np.eye(128, dtype=np.float32)
    c['ones'] = np.ones((128, 128), np.float32)
    c['triu'] = (i[:, None] <= i[None, :]).astype(np.float32)
    c['negu'] = np.where(i[None, :] > i[:, None], NEG, 0.0).astype(np.float32)
    c['negus'] = np.where(i[None, :] >= i[:, None], NEG, 0.0).astype(np.float32)
    c['negl'] = np.where(i[None, :] < i[:, None], NEG, 0.0).astype(np.float32)
    c['cmask'] = np.where((i[:, None] < 64) & (i[None, :] >= 64), NEG, 0.0).astype(np.float32)
    half = DIFF_DQK // 2
    inv_freq = (ROPE_THETA ** (-np.arange(half, dtype=np.float32) / half)).astype(np.float32)
    ang = np.arange(SEQ, dtype=np.float32)[None, :] * inv_freq[:, None]
    cos = np.cos(ang).astype(np.float32)
    sin = np.sin(ang).astype(np.float32)
    c['rcos'] = np.tile(cos, (4, 1)).astype(np.float32)
    c['rsin'] = np.concatenate([-sin, sin, -sin, sin], 0).astype(np.float32)
    return c


CONST_SHAPES = dict(ident=[128, 128], ones=[128, 128], triu=[128, 128], negu=[128, 128], negus=[128, 128],
                    negl=[128, 128], cmask=[128, 128], rcos=[128, SEQ], rsin=[128, SEQ])

WEIGHT_SHAPES = dict(
    w_in=[DEPTH, D_MODEL, D_IN], gdn_conv_w=[DEPTH, 4, 1536], gdn_a_log=[DEPTH, 4], gdn_dt_bias=[DEPTH, 4],
    gdn_norm_w=[DEPTH, 128], diff_lambda=[DEPTH, 4, 64], diff_norm_w=[DEPTH, 128],
    ssm_conv_w=[DEPTH, 4, 1024], ssm_conv_b=[DEPTH, 1024], ssm_a_log=[DEPTH, 8], ssm_dt_bias=[DEPTH, 8],
    ssm_d=[DEPTH, 8], ssm_norm_w=[DEPTH, 512], w_mem=[DEPTH, D_MODEL, 1024],
    w_branch=[DEPTH, 4, 512, D_MODEL], w_out=[DEPTH, D_MODEL, D_MODEL], ln1_g=[DEPTH, D_MODEL],
    ln1_b=[DEPTH, D_MODEL], router_w=[DEPTH, D_MODEL, N_EXPERTS], router_b=[DEPTH, N_EXPERTS],
    w_gate_up=[DEPTH, N_EXPERTS, D_MODEL, 2 * D_FF], b_gate_up=[DEPTH, N_EXPERTS, 2 * D_FF],
    w_down=[DEPTH, N_EXPERTS, D_FF, D_MODEL], b_down=[DEPTH, N_EXPERTS, D_MODEL],
    ln2_g=[DEPTH, D_MODEL], ln2_b=[DEPTH, D_MODEL])


class T:
    __slots__ = ('ap', 'k')

    def __init__(self, ap, k):
        self.ap = ap
        self.k = k


class Kern:
    ARENA_F32 = 52992

    def __init__(self, nseq, layers, moe_experts=N_EXPERTS):
        self.nseq = nseq
        self.dbg = {}
        self.layers = layers
        self.moe_experts = moe_experts
        nc = self.nc = bass.Bass("TRN2", target_bir_lowering=False)
        self.s = Sched(nc)
        d = self.d = {}
        nl = len(layers)
        d['x'] = nc.dram_tensor("x", [nseq, SEQ, D_MODEL], F32, kind="ExternalInput").ap()
        d['mem'] = nc.dram_tensor("mem", [nseq, MEM_LEN, D_MODEL], F32, kind="ExternalInput").ap()
        for n, sh in WEIGHT_SHAPES.items():
            sh = [nl] + list(sh[1:])
            if n in ('w_gate_up', 'w_down'):
                sh[1] = moe_experts
            d[n] = nc.dram_tensor(n, sh, F32, kind="ExternalInput").ap()
        for n, sh in CONST_SHAPES.items():
            d['c_' + n] = nc.dram_tensor('c_' + n, sh, F32, kind="ExternalInput").ap()
        d['y'] = nc.dram_tensor("y", [nseq, SEQ, D_MODEL], F32, kind="ExternalOutput").ap()
        d['xs'] = nc.dram_tensor("xs", [2, SEQ, D_MODEL], F32, kind="Internal").ap()
        self.arena = nc.alloc_sbuf_tensor("arena", [128, self.ARENA_F32], F32).ap()
        self.top = 0
        self.uid = 0
        self.psum = nc.alloc_psum_tensor("psum", [128, 4096], F32).ap()
        self.psum_bf = self.psum.bitcast(BF16)
        self.bank_rr = 0
        self.pair_rr = 0
        self.bank_lim = 8

    def alloc(self, shape, dtype=F32, name="t"):
        esz = 4 if dtype == F32 else 2
        n = 1
        for v in shape[1:]:
            n *= v
        nb = (n * esz + 31) // 32 * 32
        n4 = nb // 4
        if self.top + n4 > self.ARENA_F32:
            raise RuntimeError(f"arena overflow allocating {name} {shape}: top={self.top*4} need={nb}")
        ap = self.arena[:shape[0], self.top:self.top + n4]
        if dtype != F32:
            ap = ap.bitcast(dtype)
        ap = ap[:, :n]
        if len(shape) == 3:
            ap = ap.rearrange("p (a b) -> p a b", a=shape[1])
        elif len(shape) == 4:
            ap = ap.rearrange("p (a b c) -> p a b c", a=shape[1], b=shape[2])
        self.top += n4
        self.uid += 1
        return T(ap, f"{name}#{self.uid}")

    @contextmanager
    def scope(self):
        mark = self.top
        yield
        self.s.barrier()
        self.top = mark

    def bank(self):
        i = self.bank_rr % self.bank_lim
        self.bank_rr += 1
        return i

    def pair(self):
        i = (self.pair_rr % 4) * 2
        self.pair_rr += 1
        return i

    def pb(self, i, n=512, off=0):
        return self.psum[:, i * 512 + off:i * 512 + off + n]

    def pbb(self, i, n=1024, off=0):
        return self.psum_bf[:, i * 1024 + off:i * 1024 + off + n]

    @staticmethod
    def pk(i):
        return ('ps', i)

    def MM(self, out, lhsT, rhs, start, stop, r, w):
        return self.s.op('pe', lambda e: e.matmul(out, lhsT=lhsT, rhs=rhs, start=start, stop=stop), r, w)

    def TR(self, out, in_, ident, r, w):
        return self.s.op('pe', lambda e: e.transpose(out, in_, ident), r, w)

    def ACT(self, out, in_, func, r, w, bias=None, scale=None, accum=None, eng='act'):
        kw = {}
        if bias is not None:
            kw['bias'] = bias
        if scale is not None:
            kw['scale'] = scale
        if accum is not None:
            kw['accum_out'] = accum
        return self.s.op(eng, lambda e: e.activation(out=out, in_=in_, func=func, **kw), r, w)

    def TS(self, e, out, in0, s1, s2, op0, op1, r, w, accum=None):
        kw = {}
        if accum is not None:
            kw['accum_out'] = accum
        if op1 is None:
            return self.s.op(e, lambda g: g.tensor_scalar(out=out, in0=in0, scalar1=s1, scalar2=None, op0=op0, **kw), r, w)
        return self.s.op(e, lambda g: g.tensor_scalar(out=out, in0=in0, scalar1=s1, scalar2=s2, op0=op0, op1=op1, **kw), r, w)

    def TT(self, e, out, in0, in1, op, r, w):
        return self.s.op(e, lambda g: g.tensor_tensor(out=out, in0=in0, in1=in1, op=op), r, w)

    def STT(self, out, in0, sc, in1, op0, op1, r, w):
        return self.s.op('dve', lambda g: g.scalar_tensor_tensor(out=out, in0=in0, scalar=sc, in1=in1, op0=op0, op1=op1), r, w)

    def CP(self, e, out, in_, r, w):
        if e == 'act':
            return self.s.op('act', lambda g: g.activation(out=out, in_=in_, func=AF.Copy), r, w)
        return self.s.op(e, lambda g: g.tensor_copy(out=out, in_=in_), r, w)

    def DMA(self, q, out, in_, r, w, **kw):
        return self.s.dma_op(q, out, in_, r, w, **kw)

    def load_consts(self):
        self.c = {}
        for n in ('ident', 'ones', 'triu', 'negu', 'negus', 'negl', 'cmask'):
            t = self.alloc([128, 128], F32, 'c_' + n)
            self.DMA('sp', t.ap, self.d['c_' + n], [], [t.k])
            self.c[n] = t
        t = self.alloc([128, 128], BF16, 'c_identb')
        self.CP('dve', t.ap, self.c['ident'].ap, [self.c['ident'].k], [t.k])
        self.c['identb'] = t

    def to_fm(self, src_ap, src_k, F, tile, f32copy=None):
        idt = self.c['ident']
        for half in range(2):
            b = self.bank()
            for j in range(4):
                kc = half * 4 + j
                self.TR(self.pb(b, 128, j * 128), src_ap[:, kc * 128:(kc + 1) * 128], idt.ap,
                        [src_k, idt.k], [self.pk(b)])
            src = self.pb(b).rearrange("p (a b) -> p a b", a=4)
            self.s.op('act', lambda g: g.activation(out=F.ap[:, half * 4:half * 4 + 4, tile * 128:(tile + 1) * 128],
                                                    in_=src, func=AF.Copy),
                      [self.pk(b)], [(F.k, tile)])
            if f32copy is not None:
                self.CP('dve', f32copy.ap[:, half * 4:half * 4 + 4, :], src, [self.pk(b)], [f32copy.k])

    def layer_norm(self, src, src_k, dst, dst_k, grow, brow, tmp):
        st = tmp.ap[:, 0:12].rearrange("p (a b) -> p a b", a=2)
        for h in range(2):
            self.s.op('dve', lambda g: g.bn_stats(out=st[:, h, :], in_=src[:, h * 512:(h + 1) * 512]), [src_k], [tmp.k])
        mv = tmp.ap[:, 12:14]
        self.s.op('dve', lambda g: g.bn_aggr(out=mv, in_=tmp.ap[:, 0:12]), [tmp.k], [tmp.k])
        self.TS('dve', tmp.ap[:, 14:15], mv[:, 1:2], LN_EPS, None, ALU.add, None, [tmp.k], [tmp.k])
        self.ACT(tmp.ap[:, 14:15], tmp.ap[:, 14:15], AF.Sqrt, [tmp.k], [tmp.k])
        self.s.op('dve', lambda g: g.reciprocal(out=tmp.ap[:, 15:16], in_=tmp.ap[:, 14:15]), [tmp.k], [tmp.k])
        self.TS('dve', dst, src, mv[:, 0:1], tmp.ap[:, 15:16], ALU.subtract, ALU.mult, [src_k, tmp.k], [dst_k])
        self.TT('pool', dst, dst, grow.ap, ALU.mult, [dst_k, grow.k], [dst_k])
        self.TT('pool', dst, dst, brow.ap, ALU.add, [dst_k, brow.k], [dst_k])

    def moe_block(self, l, XA, F0, tiles, x2_dst, next_F=True):
        K = self
        d = self.d
        NTB = len(tiles)
        assert NTB % 4 == 0
        NE = self.moe_experts
        with K.scope():
            gate = K.alloc([128, NTB, 32], F32, 'gate')
            bg = K.alloc([128, 16, 32], F32, 'bg')
            with K.scope():
                rw = K.alloc([128, 8, 32], F32, 'rw')
                K.DMA('sp', rw.ap, d['router_w'][l].rearrange("(c p) n -> p c n", p=128), [], [rw.k])
                rb = K.alloc([128, 32], F32, 'rb')
                K.DMA('sp', rb.ap, d['router_b'][l].partition_broadcast(128), [], [rb.k])
                bdn = K.alloc([128, 1024], F32, 'bdn')
                K.s.op('pool', lambda g: g.memset(bdn.ap, 0.0), [], [bdn.k])
                K.DMA('sp', bdn.ap[:32, :], d['b_down'][l], [], [bdn.k])
                gpad = K.alloc([128, 128], F32, 'gpad')
                K.s.op('pool', lambda g: g.memset(gpad.ap, 0.0), [], [gpad.k])
                bgr = K.alloc([32, 2048], F32, 'bgr')
                K.DMA('sp', bgr.ap, d['b_gate_up'][l], [], [bgr.k])
                idt = K.c['ident']
                b = K.bank()
                for c in range(16):
                    par = c // 8
                    cc = c % 8
                    K.TR(K.pb(b, 32, c * 32), bgr.ap[:, 2 * cc * 128 + par:2 * (cc + 1) * 128:2], idt.ap[:32, :32],
                         [bgr.k, idt.k], [K.pk(b)])
                K.CP('dve', bg.ap.rearrange("p a b -> p (a b)"), K.pb(b), [K.pk(b)], [bg.k])
                K.TS('dve', bg.ap[:, 8:16, :], bg.ap[:, 8:16, :], 1.0, None, ALU.add, None, [bg.k], [bg.k])
                xTf = K.alloc([128, 8, 128], F32, 'xTf')
                sm = K.alloc([128, 64], F32, 'sm')
                lg = K.alloc([128, 32], F32, 'lg')
                gT = K.alloc([128, 128], F32, 'gT')
                for ti, t in enumerate(tiles if K.dbg.get('router', 1) else []):
                    xa = XA.ap[:, ti, :]
                    xk = (XA.k, ti)
                    K.to_fm(xa, xk, F0, t, f32copy=xTf)
                    RL = K.dbg.get('router', 9)
                    if RL < 2:
                        continue
                    b = K.bank()
                    for kc in range(8):
                        K.MM(K.pb(b, 32), xTf.ap[:, kc, :], rw.ap[:, kc, :], kc == 0, kc == 7, [xTf.k, rw.k], [K.pk(b)])
                    K.TT('dve', lg.ap, K.pb(b, 32), rb.ap, ALU.add, [K.pk(b), rb.k], [lg.k])
                    if RL < 3:
                        continue
                    K.s.op('dve', lambda g: g.max(out=sm.ap[:, 0:8], in_=lg.ap), [lg.k], [sm.k])
                    K.TS('dve', sm.ap[:, 8:9], sm.ap[:, 0:1], -1.0, None, ALU.mult, None, [sm.k], [sm.k])
                    K.ACT(sm.ap[:, 16:48], lg.ap, AF.Exp, [lg.k, sm.k], [sm.k], bias=sm.ap[:, 8:9])
                    K.TS('dve', lg.ap, lg.ap, sm.ap[:, 3:4], None, ALU.is_ge, None, [lg.k, sm.k], [lg.k])
                    K.TT('dve', sm.ap[:, 16:48], sm.ap[:, 16:48], lg.ap, ALU.mult, [sm.k, lg.k], [sm.k])
                    K.s.op('dve', lambda g: g.reduce_sum(out=sm.ap[:, 9:10], in_=sm.ap[:, 16:48], axis=AX.X), [sm.k], [sm.k])
                    K.s.op('dve', lambda g: g.reciprocal(out=sm.ap[:, 10:11], in_=sm.ap[:, 9:10]), [sm.k], [sm.k])
                    K.TS('dve', gate.ap[:, ti, :], sm.ap[:, 16:48], sm.ap[:, 10:11], None, ALU.mult, None, [sm.k], [(gate.k, ti)])
                    if RL < 4:
                        continue
                    K.CP('dve', gpad.ap[:, 0:32], gate.ap[:, ti, :], [(gate.k, ti)], [gpad.k])
                    b = K.bank()
                    K.TR(K.pb(b, 128), gpad.ap, idt.ap, [gpad.k, idt.k], [K.pk(b)])
                    K.CP('dve', gT.ap, K.pb(b, 128), [K.pk(b)], [gT.k])
                    p2 = K.pair()
                    for nb in range(2):
                        K.MM(K.pb(p2 + nb), gT.ap, bdn.ap[:, nb * 512:(nb + 1) * 512], True, True, [gT.k, bdn.k], [K.pk(p2 + nb)])
                    for nb in range(2):
                        K.STT(xa[:, nb * 512:(nb + 1) * 512], xa[:, nb * 512:(nb + 1) * 512], ALPHA, K.pb(p2 + nb),
                              ALU.mult, ALU.add, [xk, K.pk(p2 + nb)], [xk])
            with K.scope():
                NB = 2
                wgu = [K.alloc([128, 8, 1024], BF16, 'wgu') for _ in range(NB)]
                wd = [K.alloc([128, 4, 1024], BF16, 'wd') for _ in range(NB)]
                actT = [K.alloc([128, 4, 512], BF16, 'actT') for _ in range(2)]
                tg = [K.alloc([128, 512], F32, 'tg') for _ in range(2)]
                tl = [K.alloc([128, 512], F32, 'tl') for _ in range(2)]
                tsg = [K.alloc([128, 512], F32, 'tsg') for _ in range(2)]
                it = 0
                ia = 0
                ie = 0
                for e in range(NE if K.dbg.get('experts', 1) else 0):
                    for half in range(2):
                        wb = it % NB
                        it += 1
                        K.DMA('pool', wgu[wb].ap,
                              d['w_gate_up'][l, e][:, half * 1024:(half + 1) * 1024].rearrange("(c p) n -> p c n", p=128),
                              [], [wgu[wb].k])
                        K.DMA('pool', wd[wb].ap,
                              d['w_down'][l, e][half * 512:(half + 1) * 512, :].rearrange("(c p) n -> p c n", p=128),
                              [], [wd[wb].k])
                        for tb in range(NTB // 4):
                            at = actT[ia % 2]
                            ia += 1
                            for j in range(4):
                                c = half * 4 + j
                                p2 = K.pair()
                                for par in range(2):
                                    for kc in range(8):
                                        K.MM(K.pb(p2 + par), wgu[wb].ap[:, kc, j * 256 + par:(j + 1) * 256:2],
                                             F0.ap[:, kc, tiles[tb * 4] * 128:(tiles[tb * 4] + 4) * 128],
                                             kc == 0, kc == 7,
                                             [wgu[wb].k] + [(F0.k, tiles[tb * 4 + q]) for q in range(4)], [K.pk(p2 + par)])
                                x = ie % 2
                                ie += 1
                                K.TS('dve', tl[x].ap, K.pb(p2 + 1), bg.ap[:, 8 + c, e:e + 1], SWIGLU_LIMIT + 1.0, ALU.add, ALU.min,
                                     [K.pk(p2 + 1), bg.k], [tl[x].k])
                                K.TS('dve', tg[x].ap, K.pb(p2), bg.ap[:, c, e:e + 1], SWIGLU_LIMIT, ALU.add, ALU.min,
                                     [K.pk(p2), bg.k], [tg[x].k])
                                K.ACT(tsg[x].ap, tg[x].ap, AF.Sigmoid, [tg[x].k], [tsg[x].k], scale=SWIGLU_ALPHA)
                                K.STT(tl[x].ap, tl[x].ap, -SWIGLU_LIMIT + 1.0, tg[x].ap, ALU.max, ALU.mult,
                                      [tl[x].k, tg[x].k], [tl[x].k])
                                K.TT('pool', at.ap[:, j, :], tl[x].ap, tsg[x].ap, ALU.mult, [tl[x].k, tsg[x].k], [(at.k, j)])
                            for q in range(4):
                                ti = tb * 4 + q
                                p2 = K.pair()
                                for nb in range(2):
                                    for j in range(4):
                                        K.MM(K.pb(p2 + nb), at.ap[:, j, q * 128:(q + 1) * 128], wd[wb].ap[:, j, nb * 512:(nb + 1) * 512],
                                             j == 0, j == 3, [(at.k, j), wd[wb].k], [K.pk(p2 + nb)])
                                xa = XA.ap[:, ti, :]
                                for nb in range(2):
                                    K.STT(xa[:, nb * 512:(nb + 1) * 512], K.pb(p2 + nb), gate.ap[:, ti, e:e + 1],
                                          xa[:, nb * 512:(nb + 1) * 512], ALU.mult, ALU.add,
                                          [K.pk(p2 + nb), (gate.k, ti), (XA.k, ti)], [(XA.k, ti)])
            with K.scope():
                g2 = K.alloc([128, 1024], F32, 'g2')
                b2 = K.alloc([128, 1024], F32, 'b2')
                K.DMA('sp', g2.ap, d['ln2_g'][l].partition_broadcast(128), [], [g2.k])
                K.DMA('sp', b2.ap, d['ln2_b'][l].partition_broadcast(128), [], [b2.k])
                tmp = K.alloc([128, 16], F32, 'lntmp')
                for ti, t in enumerate(tiles):
                    xa = XA.ap[:, ti, :]
                    xk = (XA.k, ti)
                    if K.dbg.get('ln2', 1):
                        K.layer_norm(xa, xk, xa, xk, g2, b2, tmp)
                    K.DMA('sp', x2_dst(t), xa, [xk], [('x2', t)])
                    if next_F:
                        K.to_fm(xa, xk, F0, t)


    def load_w(self, src, nchunks, ncols, name='w'):
        t = self.alloc([128, nchunks, ncols], BF16, name)
        self.DMA('pool', t.ap, src.rearrange("(c p) n -> p c n", p=128), [], [t.k])
        return t

    def load_row(self, src, n, name='row', parts=128):
        t = self.alloc([parts, n], F32, name)
        self.DMA('sp', t.ap, src.partition_broadcast(parts), [], [t.k])
        return t

    def fkeys(self, F, blk):
        return [(F.k, blk * 4 + q) for q in range(4)]

    def br_mem(self, l, F0, YT, memT):
        K = self
        d = self.d
        scale = 128.0 ** -0.5
        Wm = K.load_w(d['w_mem'][l], 8, 1024, 'Wm')
        Wq = K.load_w(d['w_in'][l][:, C_MQ:C_MQ + 512], 8, 512, 'Wq')
        kmT = K.alloc([128, 4, 256], BF16, 'kmT')
        vm = K.alloc([128, 2, 512], BF16, 'vm')
        mk = [(memT.k, 0), (memT.k, 1)]
        for h in range(4):
            b = K.bank()
            for kc in range(8):
                K.MM(K.pb(b, 256), Wm.ap[:, kc, h * 128:(h + 1) * 128], memT.ap[:, kc, :], kc == 0, kc == 7,
                     [Wm.k] + mk, [K.pk(b)])
            K.CP('act', kmT.ap[:, h, :], K.pb(b, 256), [K.pk(b)], [kmT.k])
        for mt in range(2):
            b = K.bank()
            for kc in range(8):
                K.MM(K.pb(b), memT.ap[:, kc, mt * 128:(mt + 1) * 128], Wm.ap[:, kc, 512:1024], kc == 0, kc == 7,
                     [Wm.k] + mk, [K.pk(b)])
            K.CP('act', vm.ap[:, mt, :], K.pb(b), [K.pk(b)], [vm.k])
        qT = K.alloc([128, 4, SEQ], BF16, 'qT')
        for h in range(4):
            for blk in range(4):
                b = K.bank()
                for kc in range(8):
                    K.MM(K.pb(b), Wq.ap[:, kc, h * 128:(h + 1) * 128], F0.ap[:, kc, blk * 512:(blk + 1) * 512],
                         kc == 0, kc == 7, [Wq.k] + K.fkeys(F0, blk), [K.pk(b)])
                K.CP('act', qT.ap[:, h, blk * 512:(blk + 1) * 512], K.pb(b), [K.pk(b)], [(qT.k, h, blk)])
        P = [K.alloc([128, 4, 256], F32, 'P') for _ in range(2)]
        Pn = [K.alloc([128, 4, 256], BF16, 'Pn') for _ in range(2)]
        PT = [K.alloc([128, 8, 128], BF16, 'PT') for _ in range(2)]
        st = [K.alloc([128, 16], F32, 'st') for _ in range(2)]
        idb = K.c['identb']
        for t in range(SEQ // 128):
            x = t % 2
            blk = t // 4
            p2 = K.pair()
            for h in range(4):
                K.MM(K.psum[:, p2 * 512 + h * 256:p2 * 512 + (h + 1) * 256], qT.ap[:, h, t * 128:(t + 1) * 128], kmT.ap[:, h, :],
                     True, True, [(qT.k, h, blk), kmT.k], [K.pk(p2 + h // 2)])
            S3 = K.psum[:, p2 * 512:p2 * 512 + 1024].rearrange("p (a b) -> p a b", a=4)
            pk2 = [K.pk(p2), K.pk(p2 + 1)]
            K.s.op('dve', lambda g: g.tensor_reduce(out=st[x].ap[:, 0:4], in_=S3, axis=AX.X, op=ALU.max), pk2, [st[x].k])
            K.TS('dve', st[x].ap[:, 4:8], st[x].ap[:, 0:4], -scale, None, ALU.mult, None, [st[x].k], [st[x].k])
            for h in range(4):
                K.ACT(P[x].ap[:, h, :], S3[:, h, :], AF.Exp, [K.pk(p2 + h // 2), st[x].k], [P[x].k, st[x].k],
                      bias=st[x].ap[:, 4 + h:5 + h], scale=scale, accum=st[x].ap[:, 8 + h:9 + h])
            K.s.op('dve', lambda g: g.reciprocal(out=st[x].ap[:, 12:16], in_=st[x].ap[:, 8:12]), [st[x].k], [st[x].k])
            K.TT('dve', Pn[x].ap, P[x].ap, st[x].ap[:, 12:16].unsqueeze(2).broadcast_to([128, 4, 256]), ALU.mult,
                 [P[x].k, st[x].k], [Pn[x].k])
            b = K.bank()
            for h in range(4):
                for mt in range(2):
                    K.TR(K.pbb(b, 128, (h * 2 + mt) * 128), Pn[x].ap[:, h, mt * 128:(mt + 1) * 128], idb.ap,
                         [Pn[x].k, idb.k], [K.pk(b)])
            K.CP('act', PT[x].ap.rearrange("p a b -> p (a b)"), K.pbb(b), [K.pk(b)], [PT[x].k])
            b = K.bank()
            for h in range(4):
                for mt in range(2):
                    K.MM(K.pb(b, 128, h * 128), vm.ap[:, mt, h * 128:(h + 1) * 128], PT[x].ap[:, h * 2 + mt, :],
                         mt == 0, mt == 1, [vm.k, PT[x].k], [K.pk(b)])
            K.CP('dve', YT.ap[:, :, t * 128:(t + 1) * 128], K.pb(b).rearrange("p (a b) -> p a b", a=4), [K.pk(b)],
                 [(YT.k, h, blk) for h in range(4)])

    def br_diff(self, l, F0, YT, memT):
        K = self
        d = self.d
        gl = self.layers[l]
        scale = 64.0 ** -0.5
        lam_init = 0.8 - 0.6 * math.exp(-0.3 * gl)
        NT = SEQ // 128
        qT = K.alloc([128, 4, SEQ], BF16, 'dqT')
        kT = K.alloc([128, 4, SEQ], BF16, 'dkT')
        Vt = K.alloc([128, NT, 512], BF16, 'dVt')
        qk = (qT, kT)
        with K.scope():
            Wqk = K.load_w(d['w_in'][l][:, C_DQ:C_DQ + 1024], 8, 1024, 'Wqk')
            Wv = K.load_w(d['w_in'][l][:, C_DV:C_DV + 512], 8, 512, 'Wv')
            Wsw = K.alloc([128, 8, 1024], BF16, 'Wsw')
            v5 = Wqk.ap.rearrange("p c (m two h) -> p c m two h", two=2, h=32)
            w5 = Wsw.ap.rearrange("p c (m two h) -> p c m two h", two=2, h=32)
            K.CP('dve', w5[:, :, :, 0, :], v5[:, :, :, 1, :], [Wqk.k], [Wsw.k])
            K.CP('pool', w5[:, :, :, 1, :], v5[:, :, :, 0, :], [Wqk.k], [Wsw.k])
            rc = K.alloc([128, SEQ], F32, 'rc')
            rs = K.alloc([128, SEQ], F32, 'rs')
            K.DMA('sp', rc.ap, d['c_rcos'], [], [rc.k])
            K.DMA('sp', rs.ap, d['c_rsin'], [], [rs.k])
            t1 = [K.alloc([128, 512], F32, 't1') for _ in range(2)]
            t2 = [K.alloc([128, 512], F32, 't2') for _ in range(2)]
            n = 0
            for w in range(2):
                for h in range(4):
                    c0 = w * 512 + h * 128
                    for blk in range(4):
                        x = n % 2
                        n += 1
                        bA = K.bank()
                        for kc in range(8):
                            K.MM(K.pb(bA), Wqk.ap[:, kc, c0:c0 + 128], F0.ap[:, kc, blk * 512:(blk + 1) * 512],
                                 kc == 0, kc == 7, [Wqk.k] + K.fkeys(F0, blk), [K.pk(bA)])
                        bB = K.bank()
                        for kc in range(8):
                            K.MM(K.pb(bB), Wsw.ap[:, kc, c0:c0 + 128], F0.ap[:, kc, blk * 512:(blk + 1) * 512],
                                 kc == 0, kc == 7, [Wsw.k] + K.fkeys(F0, blk), [K.pk(bB)])
                        K.TT('dve', t1[x].ap, K.pb(bA), rc.ap[:, blk * 512:(blk + 1) * 512], ALU.mult, [K.pk(bA), rc.k], [t1[x].k])
                        K.TT('dve', t2[x].ap, K.pb(bB), rs.ap[:, blk * 512:(blk + 1) * 512], ALU.mult, [K.pk(bB), rs.k], [t2[x].k])
                        K.TT('pool', qk[w].ap[:, h, blk * 512:(blk + 1) * 512], t1[x].ap, t2[x].ap, ALU.add,
                             [t1[x].k, t2[x].k], [(qk[w].k, h, blk)])
            for t in range(NT):
                b = K.bank()
                for kc in range(8):
                    K.MM(K.pb(b), F0.ap[:, kc, t * 128:(t + 1) * 128], Wv.ap[:, kc, :], kc == 0, kc == 7,
                         [Wv.k, (F0.k, t)], [K.pk(b)])
                K.CP('act', Vt.ap[:, t, :], K.pb(b), [K.pk(b)], [(Vt.k, t)])
        lp = K.alloc([128, 256], F32, 'lp')
        K.DMA('sp', lp.ap, d['diff_lambda'][l].rearrange("a b -> (a b)").partition_broadcast(128), [], [lp.k])
        lt = K.alloc([128, 144], F32, 'lt')
        lp4 = lp.ap.rearrange("p (a b c) -> p a b c", a=2, b=2)
        K.TT('dve', lt.ap[:, 0:128].rearrange("p (a c) -> p a c", a=2), lp4[:, :, 0, :], lp4[:, :, 1, :], ALU.mult, [lp.k], [lt.k])
        K.s.op('dve', lambda g: g.tensor_reduce(out=lt.ap[:, 128:130], in_=lt.ap[:, 0:128].rearrange("p (a c) -> p a c", a=2),
                                                 axis=AX.X, op=ALU.add), [lt.k], [lt.k])
        K.ACT(lt.ap[:, 130:132], lt.ap[:, 128:130], AF.Exp, [lt.k], [lt.k])
        K.TS('dve', lt.ap[:, 132:133], lt.ap[:, 130:131], lt.ap[:, 131:132], lam_init, ALU.subtract, ALU.add, [lt.k], [lt.k])
        K.TS('dve', lt.ap[:, 133:134], lt.ap[:, 132:133], -1.0, None, ALU.mult, None, [lt.k], [lt.k])
        nlam = lt.ap[:, 133:134]
        nw = K.load_row(d['diff_norm_w'][l], 128, 'dnw')
        K.TS('dve', nw.ap, nw.ap, 1.0 - lam_init, None, ALU.mult, None, [nw.k], [nw.k])
        Pb = [K.alloc([128, SEQ], BF16, 'Pb') for _ in range(2)]
        PT = [K.alloc([128, NT, 128], BF16, 'PT') for _ in range(2)]
        st = [K.alloc([128, 16], F32, 'dst') for _ in range(2)]
        ot = [K.alloc([128, 512], F32, 'ot') for _ in range(2)]
        sq = K.alloc([128, 512], F32, 'sq')
        yb = [K.alloc([128, 512], BF16, 'yb') for _ in range(2)]
        rr = K.alloc([128, 16], F32, 'rr')
        cm = K.c['cmask']
        idb = K.c['identb']
        it = 0
        io = 0
        K.bank_lim = 6
        for t in range(NT):
            kend = (t + 1) * 128
            nkb = (kend + 511) // 512
            o = ot[t % 2]
            for h in range(4):
                bO = 6 + io % 2
                io += 1
                rcp = []
                for m in range(2):
                    x = it % 2
                    it += 1
                    sx = st[x]
                    banks = []
                    for kb in range(nkb):
                        w = min(512, kend - kb * 512)
                        b = K.bank()
                        banks.append((b, w))
                        K.MM(K.pb(b, w), qT.ap[m * 64:(m + 1) * 64, h, t * 128:(t + 1) * 128],
                             kT.ap[m * 64:(m + 1) * 64, h, kb * 512:kb * 512 + w], True, True,
                             [(qT.k, h, t // 4), (kT.k, h, kb)], [K.pk(b)])
                        if kb == nkb - 1:
                            K.TT('dve', K.pb(b, 128, w - 128), K.pb(b, 128, w - 128), cm.ap, ALU.add, [K.pk(b), cm.k], [K.pk(b)])
                        K.s.op('dve', lambda g: g.tensor_reduce(out=sx.ap[:, kb:kb + 1], in_=K.pb(b, w), axis=AX.X, op=ALU.max),
                               [K.pk(b)], [sx.k])
                    if nkb > 1:
                        K.s.op('dve', lambda g: g.tensor_reduce(out=sx.ap[:, 4:5], in_=sx.ap[:, 0:nkb], axis=AX.X, op=ALU.max),
                               [sx.k], [sx.k])
                        mxa = sx.ap[:, 4:5]
                    else:
                        mxa = sx.ap[:, 0:1]
                    K.TS('dve', sx.ap[:, 5:6], mxa, -scale, None, ALU.mult, None, [sx.k], [sx.k])
                    for kb, (b, w) in enumerate(banks):
                        K.ACT(Pb[x].ap[:, kb * 512:kb * 512 + w], K.pb(b, w), AF.Exp, [K.pk(b), sx.k], [Pb[x].k, sx.k],
                              bias=sx.ap[:, 5:6], scale=scale, accum=sx.ap[:, 8 + kb:9 + kb])
                    if nkb > 1:
                        K.s.op('dve', lambda g: g.tensor_reduce(out=sx.ap[:, 6:7], in_=sx.ap[:, 8:8 + nkb], axis=AX.X, op=ALU.add),
                               [sx.k], [sx.k])
                        sma = sx.ap[:, 6:7]
                    else:
                        sma = sx.ap[:, 8:9]
                    K.s.op('dve', lambda g: g.reciprocal(out=rr.ap[:, m:m + 1], in_=sma), [sx.k], [rr.k])
                    for j0 in range(0, t + 1, 8):
                        nj = min(8, t + 1 - j0)
                        b = K.bank()
                        for j in range(j0, j0 + nj):
                            K.TR(K.pbb(b, 128, (j - j0) * 128), Pb[x].ap[:, j * 128:(j + 1) * 128], idb.ap, [Pb[x].k, idb.k], [K.pk(b)])
                        K.CP('act', PT[x].ap[:, j0:j0 + nj, :].rearrange("p a b -> p (a b)"), K.pbb(b, nj * 128), [K.pk(b)], [PT[x].k])
                    for j in range(t + 1):
                        K.MM(K.pb(bO, 128, m * 128), PT[x].ap[:, j, :], Vt.ap[:, j, h * 128:(h + 1) * 128], j == 0, j == t,
                             [PT[x].k, (Vt.k, j)], [K.pk(bO)])
                K.TS('dve', rr.ap[:, 1:2], rr.ap[:, 1:2], nlam, None, ALU.mult, None, [rr.k, lt.k], [rr.k])
                oh = o.ap[:, h * 128:(h + 1) * 128]
                K.TS('dve', oh, K.pb(bO, 128, 0), rr.ap[:, 0:1], None, ALU.mult, None, [K.pk(bO), rr.k], [o.k])
                K.STT(oh, K.pb(bO, 128, 128), rr.ap[:, 1:2], oh, ALU.mult, ALU.add, [K.pk(bO), rr.k, o.k], [o.k])
            K.ACT(sq.ap, o.ap, AF.Square, [o.k], [sq.k])
            K.s.op('dve', lambda g: g.tensor_reduce(out=rr.ap[:, 4:8], in_=sq.ap.rearrange("p (a b) -> p a b", a=4), axis=AX.X, op=ALU.add),
                   [sq.k], [rr.k])
            K.TS('dve', rr.ap[:, 4:8], rr.ap[:, 4:8], 1.0 / 128.0, NORM_EPS, ALU.mult, ALU.add, [rr.k], [rr.k])
            K.ACT(rr.ap[:, 4:8], rr.ap[:, 4:8], AF.Sqrt, [rr.k], [rr.k])
            K.s.op('dve', lambda g: g.reciprocal(out=rr.ap[:, 8:12], in_=rr.ap[:, 4:8]), [rr.k], [rr.k])
            o3 = o.ap.rearrange("p (a b) -> p a b", a=4)
            K.TT('dve', o3, o3, rr.ap[:, 8:12].unsqueeze(2).broadcast_to([128, 4, 128]), ALU.mult, [o.k, rr.k], [o.k])
            y = yb[t % 2]
            K.TT('dve', y.ap.rearrange("p (a b) -> p a b", a=4), o3, nw.ap.unsqueeze(1).broadcast_to([128, 4, 128]), ALU.mult,
                 [o.k, nw.k], [y.k])
            b = K.bank()
            for h in range(4):
                K.TR(K.pbb(b, 128, h * 128), y.ap[:, h * 128:(h + 1) * 128], idb.ap, [y.k, idb.k], [K.pk(b)])
            K.CP('act', YT.ap[:, :, t * 128:(t + 1) * 128], K.pbb(b, 512).rearrange("p (a b) -> p a b", a=4), [K.pk(b)],
                 [(YT.k, h, t // 4) for h in range(4)])
        K.bank_lim = 8

    def softplus_rows(self, out, in_ps, bias_row, tmp, keys_r, key_w):
        K = self
        n = out.shape[1]
        xb = tmp.ap[:, 0:n]
        ax = tmp.ap[:, n:2 * n]
        K.TT('dve', xb, in_ps, bias_row.ap, ALU.add, keys_r + [bias_row.k], [tmp.k])
        K.ACT(ax, xb, AF.Abs, [tmp.k], [tmp.k])
        K.ACT(ax, ax, AF.Exp, [tmp.k], [tmp.k], scale=-1.0)
        K.ACT(ax, ax, AF.Ln, [tmp.k], [tmp.k], bias=1.0)
        K.TS('dve', xb, xb, 0.0, None, ALU.max, None, [tmp.k], [tmp.k])
        K.TT('dve', out, xb, ax, ALU.add, [tmp.k], [key_w])

    def load_T(self, src2d, nrows, name):
        K = self
        ncol = src2d.shape[1]
        c = ncol // 128
        with K.scope():
            raw = K.alloc([nrows, ncol], F32, name + '_raw')
            K.DMA('sp', raw.ap, src2d, [], [raw.k])
            idt = K.c['ident']
            b = K.bank()
            for i in range(c):
                K.TR(K.pb(b, nrows, i * nrows), raw.ap[:, i * 128:(i + 1) * 128], idt.ap[:nrows, :nrows], [raw.k, idt.k], [K.pk(b)])
            K.CP('dve', self._loadT_dst.ap.rearrange("p a b -> p (a b)"), K.pb(b, c * nrows), [K.pk(b)], [self._loadT_dst.k])

    def br_ssd(self, l, F0, YT, memT):
        K = self
        d = self.d
        NT = SEQ // 128
        xs_tm = K.alloc([128, NT, 512], BF16, 'xs_tm')
        B_tm = K.alloc([128, NT, 256], BF16, 'B_tm')
        BT = K.alloc([128, 2, SEQ], BF16, 'BT')
        CT = K.alloc([128, 2, SEQ], BF16, 'CT')
        idb = K.c['identb']
        cw = K.alloc([128, 8, 4], F32, 'cw')
        K._loadT_dst = cw
        K.load_T(d['ssm_conv_w'][l], 4, 'cw')
        cb = K.alloc([128, 8, 1], F32, 'cb')
        K._loadT_dst = cb
        K.load_T(d['ssm_conv_b'][l].rearrange("(o n) -> o n", o=1), 1, 'cb')
        with K.scope():
            Wx = K.load_w(d['w_in'][l][:, C_SXBC:C_SXBC + 1024], 8, 1024, 'Wx')
            xr = [K.alloc([128, SEQ + 3], F32, 'xr') for _ in range(2)]
            acc = [K.alloc([128, SEQ], F32, 'acc') for _ in range(2)]
            sb = [K.alloc([128, SEQ], BF16, 'sb') for _ in range(2)]
            for x in range(2):
                K.s.op('pool', lambda g: g.memset(xr[x].ap[:, 0:3], 0.0), [], [xr[x].k])
            for c in range(8):
                x = c % 2
                for blk in range(4):
                    b = K.bank()
                    for kc in range(8):
                        K.MM(K.pb(b), Wx.ap[:, kc, c * 128:(c + 1) * 128], F0.ap[:, kc, blk * 512:(blk + 1) * 512],
                             kc == 0, kc == 7, [Wx.k] + K.fkeys(F0, blk), [K.pk(b)])
                    K.CP('act', xr[x].ap[:, 3 + blk * 512:3 + (blk + 1) * 512], K.pb(b), [K.pk(b)], [xr[x].k])
                a = acc[x]
                K.TS('dve', a.ap, xr[x].ap[:, 3:SEQ + 3], cw.ap[:, c, 3:4], cb.ap[:, c, 0:1], ALU.mult, ALU.add,
                     [xr[x].k, cw.k, cb.k], [a.k])
                for j in (2, 1, 0):
                    K.STT(a.ap, xr[x].ap[:, j:SEQ + j], cw.ap[:, c, j:j + 1], a.ap, ALU.mult, ALU.add, [xr[x].k, cw.k, a.k], [a.k])
                if c < 4:
                    dst, dk = sb[x].ap, sb[x].k
                elif c < 6:
                    dst, dk = BT.ap[:, c - 4, :], (BT.k, c - 4)
                else:
                    dst, dk = CT.ap[:, c - 6, :], (CT.k, c - 6)
                K.ACT(dst, a.ap, AF.Silu, [a.k], [dk])
                if c < 6:
                    src = sb[x].ap if c < 4 else BT.ap[:, c - 4, :]
                    srck = sb[x].k if c < 4 else (BT.k, c - 4)
                    for t0 in range(0, NT, 8):
                        b = K.bank()
                        for t in range(t0, t0 + 8):
                            K.TR(K.pbb(b, 128, (t - t0) * 128), src[:, t * 128:(t + 1) * 128], idb.ap, [srck, idb.k], [K.pk(b)])
                        if c < 4:
                            K.CP('act', xs_tm.ap[:, t0:t0 + 8, c * 128:(c + 1) * 128],
                                 K.pbb(b).rearrange("p (a b) -> p a b", a=8), [K.pk(b)], [xs_tm.k])
                        else:
                            K.CP('act', B_tm.ap[:, t0:t0 + 8, (c - 4) * 128:(c - 3) * 128],
                                 K.pbb(b).rearrange("p (a b) -> p a b", a=8), [K.pk(b)], [B_tm.k])
        Wz = K.load_w(d['w_in'][l][:, C_SZ:C_SZ + 512], 8, 512, 'Wz')
        Wdt = K.load_w(d['w_in'][l][:, C_SDT:C_SDT + 8], 8, 8, 'Wdt')
        dtb = K.load_row(d['ssm_dt_bias'][l], 8, 'dtb')
        nA = K.load_row(d['ssm_a_log'][l], 8, 'nA')
        K.ACT(nA.ap, nA.ap, AF.Exp, [nA.k], [nA.k])
        K.TS('dve', nA.ap, nA.ap, -1.0, None, ALU.mult, None, [nA.k], [nA.k])
        Dr = K.load_row(d['ssm_d'][l], 8, 'Dr')
        Dx = K.alloc([128, 8, 64], F32, 'Dx')
        K.CP('dve', Dx.ap, Dr.ap.unsqueeze(2).broadcast_to([128, 8, 64]), [Dr.k], [Dx.k])
        nwr = K.load_row(d['ssm_norm_w'][l], 512, 'snw')
        Hs = K.alloc([128, 512], F32, 'Hs')
        Hb = K.alloc([128, 512], BF16, 'Hb')
        K.s.op('pool', lambda g: g.memset(Hs.ap, 0.0), [], [Hs.k])
        K.s.op('pool', lambda g: g.memset(Hb.ap, 0.0), [], [Hb.k])
        triu = K.c['triu']
        ones = K.c['ones']
        negl = K.c['negl']
        sm = [K.alloc([128, 96], F32, 'ssm_sm') for _ in range(2)]
        Eu = [K.alloc([128, 8, 128], F32, 'Eu') for _ in range(2)]
        MT = [K.alloc([128, 8, 128], BF16, 'MT') for _ in range(2)]
        xdt = [K.alloc([128, 512], BF16, 'xdt') for _ in range(2)]
        xdr = [K.alloc([128, 512], BF16, 'xdr') for _ in range(2)]
        yd = [K.alloc([128, 512], F32, 'yd') for _ in range(2)]
        yy = [K.alloc([128, 512], F32, 'yy') for _ in range(2)]
        szt = [K.alloc([128, 512], F32, 'szt') for _ in range(2)]
        ybf = [K.alloc([128, 512], BF16, 'ybf') for _ in range(2)]
        for t in range(NT):
            x = t % 2
            s_ = sm[x]
            bdt = K.bank()
            for kc in range(8):
                K.MM(K.pb(bdt, 8), F0.ap[:, kc, t * 128:(t + 1) * 128], Wdt.ap[:, kc, :], kc == 0, kc == 7, [Wdt.k, (F0.k, t)], [K.pk(bdt)])
            dt = s_.ap[:, 0:8]
            K.softplus_rows(dt, K.pb(bdt, 8), dtb, T(s_.ap[:, 64:88], s_.k), [K.pk(bdt)], s_.k)
            a = s_.ap[:, 8:16]
            K.TT('dve', a, dt, nA.ap, ALU.mult, [s_.k, nA.k], [s_.k])
            b = K.bank()
            K.MM(K.pb(b, 8, 0), triu.ap, a, True, True, [triu.k, s_.k], [K.pk(b)])
            K.MM(K.pb(b, 8, 8), ones.ap, a, True, True, [ones.k, s_.k], [K.pk(b)])
            K.CP('dve', s_.ap[:, 16:32], K.pb(b, 16), [K.pk(b)], [s_.k])
            ac = s_.ap[:, 16:24]
            al = s_.ap[:, 24:32]
            K.ACT(s_.ap[:, 32:40], ac, AF.Exp, [s_.k], [s_.k])
            K.TT('dve', s_.ap[:, 40:48], al, ac, ALU.subtract, [s_.k], [s_.k])
            K.ACT(s_.ap[:, 40:48], s_.ap[:, 40:48], AF.Exp, [s_.k], [s_.k])
            K.ACT(s_.ap[:, 48:56], al, AF.Exp, [s_.k], [s_.k])
            e_ac, e_rev, e_last = s_.ap[:, 32:40], s_.ap[:, 40:48], s_.ap[:, 48:56]
            p2 = K.pair()
            for h in range(8):
                K.MM(K.psum[:, p2 * 512 + h * 128:p2 * 512 + (h + 1) * 128], a[:, h:h + 1].broadcast_to([128, 128]), triu.ap,
                     True, True, [s_.k, triu.k], [K.pk(p2 + h // 4)])
            R3 = K.psum[:, p2 * 512:p2 * 512 + 1024].rearrange("p (a b) -> p a b", a=8)
            pk2 = [K.pk(p2), K.pk(p2 + 1)]
            E = Eu[x]
            K.TT('dve', E.ap, R3, ac.unsqueeze(2).broadcast_to([128, 8, 128]), ALU.subtract, pk2 + [s_.k], [E.k])
            K.TT('pool', E.ap, E.ap, negl.ap.unsqueeze(1).broadcast_to([128, 8, 128]), ALU.add, [E.k, negl.k], [E.k])
            K.ACT(E.ap, E.ap, AF.Exp, [E.k], [E.k])
            bc = K.bank()
            for g in range(2):
                K.MM(K.pb(bc, 128, g * 128), BT.ap[:, g, t * 128:(t + 1) * 128], CT.ap[:, g, t * 128:(t + 1) * 128], True, True,
                     [(BT.k, g), (CT.k, g)], [K.pk(bc)])
            cb4 = K.pb(bc, 256).rearrange("p (g i) -> p g i", g=2).unsqueeze(2).broadcast_to([128, 2, 4, 128])
            K.TT('dve', MT[x].ap.rearrange("p (g r) i -> p g r i", g=2), cb4, E.ap.rearrange("p (g r) i -> p g r i", g=2), ALU.mult,
                 [K.pk(bc), E.k], [MT[x].k])
            xs3 = xs_tm.ap[:, t, :].rearrange("p (h q) -> p h q", h=8)
            K.TT('dve', xdt[x].ap.rearrange("p (h q) -> p h q", h=8), xs3, dt.unsqueeze(2).broadcast_to([128, 8, 64]), ALU.mult,
                 [xs_tm.k, s_.k], [xdt[x].k])
            K.TT('pool', xdr[x].ap.rearrange("p (h q) -> p h q", h=8), xdt[x].ap.rearrange("p (h q) -> p h q", h=8),
                 e_rev.unsqueeze(2).broadcast_to([128, 8, 64]), ALU.mult, [xdt[x].k, s_.k], [xdr[x].k])
            bd = K.bank()
            for h in range(8):
                K.MM(K.pb(bd, 64, h * 64), MT[x].ap[:, h, :], xdt[x].ap[:, h * 64:(h + 1) * 64], True, True,
                     [MT[x].k, xdt[x].k], [K.pk(bd)])
            K.CP('act', yd[x].ap, K.pb(bd), [K.pk(bd)], [yd[x].k])
            bo = K.bank()
            for g in range(2):
                K.MM(K.pb(bo, 256, g * 256), CT.ap[:, g, t * 128:(t + 1) * 128], Hb.ap[:, g * 256:(g + 1) * 256], True, True,
                     [(CT.k, g), Hb.k], [K.pk(bo)])
            y = yy[x]
            y3 = y.ap.rearrange("p (h q) -> p h q", h=8)
            K.TT('dve', y3, K.pb(bo).rearrange("p (h q) -> p h q", h=8), e_ac.unsqueeze(2).broadcast_to([128, 8, 64]), ALU.mult,
                 [K.pk(bo), s_.k], [y.k])
            K.TT('pool', y.ap, y.ap, yd[x].ap, ALU.add, [y.k, yd[x].k], [y.k])
            bs = K.bank()
            for g in range(2):
                K.MM(K.pb(bs, 256, g * 256), B_tm.ap[:, t, g * 128:(g + 1) * 128], xdr[x].ap[:, g * 256:(g + 1) * 256], True, True,
                     [B_tm.k, xdr[x].k], [K.pk(bs)])
            H3 = Hs.ap.rearrange("p (h q) -> p h q", h=8)
            K.TT('dve', H3, H3, e_last.unsqueeze(2).broadcast_to([128, 8, 64]), ALU.mult, [Hs.k, s_.k], [Hs.k])
            K.TT('dve', Hs.ap, Hs.ap, K.pb(bs), ALU.add, [Hs.k, K.pk(bs)], [Hs.k])
            K.CP('pool', Hb.ap, Hs.ap, [Hs.k], [Hb.k])
            K.TT('pool', yd[x].ap.rearrange("p (h q) -> p h q", h=8), xs3, Dx.ap, ALU.mult, [xs_tm.k, Dx.k], [yd[x].k])
            K.TT('pool', y.ap, y.ap, yd[x].ap, ALU.add, [y.k, yd[x].k], [y.k])
            bz = K.bank()
            for kc in range(8):
                K.MM(K.pb(bz), F0.ap[:, kc, t * 128:(t + 1) * 128], Wz.ap[:, kc, :], kc == 0, kc == 7, [Wz.k, (F0.k, t)], [K.pk(bz)])
            K.ACT(szt[x].ap, K.pb(bz), AF.Silu, [K.pk(bz)], [szt[x].k])
            K.TT('dve', y.ap, y.ap, szt[x].ap, ALU.mult, [y.k, szt[x].k], [y.k])
            K.ACT(szt[x].ap, y.ap, AF.Square, [y.k], [szt[x].k])
            K.s.op('dve', lambda g: g.tensor_reduce(out=s_.ap[:, 56:58], in_=szt[x].ap.rearrange("p (a b) -> p a b", a=2), axis=AX.X, op=ALU.add),
                   [szt[x].k], [s_.k])
            K.TS('dve', s_.ap[:, 56:58], s_.ap[:, 56:58], 1.0 / 256.0, NORM_EPS, ALU.mult, ALU.add, [s_.k], [s_.k])
            K.ACT(s_.ap[:, 56:58], s_.ap[:, 56:58], AF.Sqrt, [s_.k], [s_.k])
            K.s.op('dve', lambda g: g.reciprocal(out=s_.ap[:, 58:60], in_=s_.ap[:, 56:58]), [s_.k], [s_.k])
            K.TT('dve', y.ap.rearrange("p (a b) -> p a b", a=2), y.ap.rearrange("p (a b) -> p a b", a=2),
                 s_.ap[:, 58:60].unsqueeze(2).broadcast_to([128, 2, 256]), ALU.mult, [y.k, s_.k], [y.k])
            K.TT('pool', ybf[x].ap, y.ap, nwr.ap, ALU.mult, [y.k, nwr.k], [ybf[x].k])
            b = K.bank()
            for c in range(4):
                K.TR(K.pbb(b, 128, c * 128), ybf[x].ap[:, c * 128:(c + 1) * 128], idb.ap, [ybf[x].k, idb.k], [K.pk(b)])
            K.CP('act', YT.ap[:, :, t * 128:(t + 1) * 128], K.pbb(b, 512).rearrange("p (a b) -> p a b", a=4), [K.pk(b)],
                 [(YT.k, c, t // 4) for c in range(4)])

    def br_gdn(self, l, F0, YT, memT):
        K = self
        d = self.d
        NT = SEQ // 128
        idb = K.c['identb']
        idt = K.c['ident']
        ones = K.c['ones']
        triu = K.c['triu']
        negus = K.c['negus']
        negl = K.c['negl']
        qT = K.alloc([128, 4, SEQ], BF16, 'gqT')
        kT = K.alloc([128, 4, SEQ], BF16, 'gkT')
        vT = K.alloc([128, 4, SEQ], BF16, 'gvT')
        dst3 = (qT, kT, vT)
        cw = K.alloc([128, 12, 4], F32, 'gcw')
        K._loadT_dst = cw
        K.load_T(d['gdn_conv_w'][l], 4, 'gcw')
        with K.scope():
            Wx = K.load_w(d['w_in'][l][:, C_GQ:C_GQ + 1536], 8, 1536, 'Wqkv')
            xr = [K.alloc([128, SEQ + 3], F32, 'gxr') for _ in range(2)]
            acc = [K.alloc([128, SEQ], F32, 'gacc') for _ in range(2)]
            t1 = [K.alloc([128, 512], F32, 'gt1') for _ in range(2)]
            for x in range(2):
                K.s.op('pool', lambda g: g.memset(xr[x].ap[:, 0:3], 0.0), [], [xr[x].k])
            n = 0
            for c in range(12):
                x = c % 2
                w, h = c // 4, c % 4
                for blk in range(4):
                    b = K.bank()
                    for kc in range(8):
                        K.MM(K.pb(b), Wx.ap[:, kc, c * 128:(c + 1) * 128], F0.ap[:, kc, blk * 512:(blk + 1) * 512],
                             kc == 0, kc == 7, [Wx.k] + K.fkeys(F0, blk), [K.pk(b)])
                    K.CP('act', xr[x].ap[:, 3 + blk * 512:3 + (blk + 1) * 512], K.pb(b), [K.pk(b)], [xr[x].k])
                a = acc[x]
                K.TS('dve', a.ap, xr[x].ap[:, 3:SEQ + 3], cw.ap[:, c, 3:4], None, ALU.mult, None, [xr[x].k, cw.k], [a.k])
                for j in (2, 1, 0):
                    K.STT(a.ap, xr[x].ap[:, j:SEQ + j], cw.ap[:, c, j:j + 1], a.ap, ALU.mult, ALU.add, [xr[x].k, cw.k, a.k], [a.k])
                if w == 2:
                    K.ACT(vT.ap[:, h, :], a.ap, AF.Silu, [a.k], [(vT.k, h)])
                    continue
                K.ACT(a.ap, a.ap, AF.Silu, [a.k], [a.k])
                for blk in range(4):
                    y = n % 2
                    n += 1
                    sl = a.ap[:, blk * 512:(blk + 1) * 512]
                    K.ACT(t1[y].ap, sl, AF.Square, [a.k], [t1[y].k])
                    b = K.bank()
                    K.MM(K.pb(b), ones.ap, t1[y].ap, True, True, [ones.k, t1[y].k], [K.pk(b)])
                    K.TS('dve', t1[y].ap, K.pb(b), NORM_EPS, None, ALU.add, None, [K.pk(b)], [t1[y].k])
                    K.ACT(t1[y].ap, t1[y].ap, AF.Sqrt, [t1[y].k], [t1[y].k])
                    K.s.op('dve', lambda g: g.reciprocal(out=t1[y].ap, in_=t1[y].ap), [t1[y].k], [t1[y].k])
                    if w == 0:
                        K.STT(qT.ap[:, h, blk * 512:(blk + 1) * 512], sl, 128.0 ** -0.5, t1[y].ap, ALU.mult, ALU.mult,
                              [a.k, t1[y].k], [(qT.k, h)])
                    else:
                        K.TT('dve', kT.ap[:, h, blk * 512:(blk + 1) * 512], sl, t1[y].ap, ALU.mult, [a.k, t1[y].k], [(kT.k, h)])
        Wz = K.load_w(d['w_in'][l][:, C_GZ:C_GZ + 512], 8, 512, 'gWz')
        Wba = K.load_w(d['w_in'][l][:, C_GB:C_GB + 8], 8, 8, 'gWba')
        dtb = K.load_row(d['gdn_dt_bias'][l], 4, 'gdtb')
        nA = K.load_row(d['gdn_a_log'][l], 4, 'gnA')
        K.ACT(nA.ap, nA.ap, AF.Exp, [nA.k], [nA.k])
        K.TS('dve', nA.ap, nA.ap, -1.0, None, ALU.mult, None, [nA.k], [nA.k])
        nw = K.load_row(d['gdn_norm_w'][l], 128, 'gnw')
        Sf = K.alloc([128, 4, 128], F32, 'gS')
        Sb = K.alloc([128, 4, 128], BF16, 'gSb')
        K.s.op('pool', lambda g: g.memset(Sf.ap, 0.0), [], [Sf.k])
        K.s.op('pool', lambda g: g.memset(Sb.ap, 0.0), [], [Sb.k])

        def f3(n_, dt_, nm):
            return [K.alloc([128, 4, 128], dt_, nm) for _ in range(n_)]
        sm = [K.alloc([128, 64], F32, 'gsm') for _ in range(2)]
        E = f3(1, F32, 'gE') * 2
        Dl = f3(2, F32, 'gDl')
        Du = f3(2, F32, 'gDu')
        eR = f3(2, F32, 'geR')
        Ab = f3(2, BF16, 'gA')
        Bb = f3(2, BF16, 'gB')
        Pf = f3(1, F32, 'gP')[0]
        Pb = f3(2, BF16, 'gPb')
        kd = f3(2, BF16, 'gkd')
        kdec = f3(2, BF16, 'gkdec')
        vb = f3(2, BF16, 'gvb')
        uu = f3(1, F32, 'gu') * 2
        wT = f3(2, BF16, 'gwT')
        qkT = f3(2, BF16, 'gqkT')
        qdT = f3(2, BF16, 'gqdT')
        vn = f3(2, BF16, 'gvn')
        of = f3(1, F32, 'go') * 2
        szt = f3(1, F32, 'gsz') * 2
        sq = f3(1, F32, 'gsq')[0]
        ybf = f3(2, BF16, 'gyb')

        def bc4(ap):
            return ap.unsqueeze(2).broadcast_to([128, 4, 128])

        def bch(ap):
            return ap.unsqueeze(1).broadcast_to([128, 4, 128])

        def ps3(b):
            return K.pb(b).rearrange("p (a b) -> p a b", a=4)

        def psb3(b):
            return K.pbb(b, 512).rearrange("p (a b) -> p a b", a=4)

        for t in range(NT):
            x = t % 2
            s_ = sm[x]
            tsl = slice(t * 128, (t + 1) * 128)
            bba = K.bank()
            for kc in range(8):
                K.MM(K.pb(bba, 8), F0.ap[:, kc, tsl], Wba.ap[:, kc, :], kc == 0, kc == 7, [Wba.k, (F0.k, t)], [K.pk(bba)])
            beta = s_.ap[:, 0:4]
            K.ACT(beta, K.pb(bba, 4), AF.Sigmoid, [K.pk(bba)], [s_.k])
            sp = s_.ap[:, 4:8]
            K.softplus_rows(sp, K.pb(bba, 4, 4), dtb, T(s_.ap[:, 48:60], s_.k), [K.pk(bba)], s_.k)
            g = s_.ap[:, 8:12]
            K.TT('dve', g, sp, nA.ap, ALU.mult, [s_.k, nA.k], [s_.k])
            b = K.bank()
            K.MM(K.pb(b, 4, 0), triu.ap, g, True, True, [triu.k, s_.k], [K.pk(b)])
            K.MM(K.pb(b, 4, 4), ones.ap, g, True, True, [ones.k, s_.k], [K.pk(b)])
            K.CP('dve', s_.ap[:, 12:20], K.pb(b, 8), [K.pk(b)], [s_.k])
            gc, gl = s_.ap[:, 12:16], s_.ap[:, 16:20]
            e_gc, e_rev, e_last, be, nbeta = s_.ap[:, 20:24], s_.ap[:, 24:28], s_.ap[:, 28:32], s_.ap[:, 32:36], s_.ap[:, 36:40]
            K.ACT(e_gc, gc, AF.Exp, [s_.k], [s_.k])
            K.TT('dve', e_rev, gl, gc, ALU.subtract, [s_.k], [s_.k])
            K.ACT(e_rev, e_rev, AF.Exp, [s_.k], [s_.k])
            K.ACT(e_last, gl, AF.Exp, [s_.k], [s_.k])
            K.TT('dve', be, beta, e_gc, ALU.mult, [s_.k], [s_.k])
            K.TS('dve', nbeta, beta, -1.0, None, ALU.mult, None, [s_.k], [s_.k])
            bR = K.bank()
            for h in range(4):
                K.MM(K.pb(bR, 128, h * 128), g[:, h:h + 1].broadcast_to([128, 128]), triu.ap, True, True, [s_.k, triu.k], [K.pk(bR)])
            K.TT('dve', E[x].ap, bc4(gc), ps3(bR), ALU.subtract, [s_.k, K.pk(bR)], [E[x].k])
            K.ACT(eR[x].ap, ps3(bR), AF.Exp, [K.pk(bR)], [eR[x].k])
            K.TT('pool', Dl[x].ap, E[x].ap, bch(negus.ap), ALU.add, [E[x].k, negus.k], [Dl[x].k])
            K.TT('pool', Du[x].ap, bch(negl.ap), E[x].ap, ALU.subtract, [E[x].k, negl.k], [Du[x].k])
            K.ACT(Dl[x].ap, Dl[x].ap, AF.Exp, [Dl[x].k], [Dl[x].k])
            K.ACT(Du[x].ap, Du[x].ap, AF.Exp, [Du[x].k], [Du[x].k])
            bK = K.bank()
            for h in range(4):
                K.MM(K.pb(bK, 128, h * 128), kT.ap[:, h, tsl], kT.ap[:, h, tsl], True, True, [(kT.k, h)], [K.pk(bK)])
            K.TT('dve', E[x].ap, ps3(bK), bc4(nbeta), ALU.mult, [K.pk(bK), s_.k], [E[x].k])
            A, B = Ab[0], Bb[0]
            K.TT('dve', A.ap, E[x].ap, Dl[x].ap, ALU.mult, [E[x].k, Dl[x].k], [A.k])
            bT = K.bank()
            for h in range(4):
                K.TR(K.pbb(bT, 128, h * 128), A.ap[:, h, :], idb.ap, [A.k, idb.k], [K.pk(bT)])
            K.CP('act', B.ap, psb3(bT), [K.pk(bT)], [B.k])
            K.TT('dve', Pf.ap, B.ap, bch(idt.ap), ALU.add, [B.k, idt.k], [Pf.k])
            Pc = Pb[0]
            K.CP('pool', Pc.ap, Pf.ap, [Pf.k], [Pc.k])
            for kk in range(6):
                A2, B2 = Ab[(kk + 1) % 2], Bb[(kk + 1) % 2]
                bA = K.bank()
                for h in range(4):
                    K.MM(K.pb(bA, 128, h * 128), B.ap[:, h, :], A.ap[:, h, :], True, True, [A.k, B.k], [K.pk(bA)])
                if kk < 5:
                    bB = K.bank()
                    for h in range(4):
                        K.MM(K.pb(bB, 128, h * 128), A.ap[:, h, :], B.ap[:, h, :], True, True, [A.k, B.k], [K.pk(bB)])
                K.CP('act', A2.ap, ps3(bA), [K.pk(bA)], [A2.k])
                if kk < 5:
                    K.CP('dve', B2.ap, ps3(bB), [K.pk(bB)], [B2.k])
                bP = K.bank()
                for h in range(4):
                    K.MM(K.pb(bP, 128, h * 128), A2.ap[:, h, :], Pc.ap[:, h, :], True, True, [A2.k, Pc.k], [K.pk(bP)])
                K.TT('dve', Pf.ap, Pf.ap, ps3(bP), ALU.add, [Pf.k, K.pk(bP)], [Pf.k])
                Pc = Pb[(kk + 1) % 2]
                K.CP('pool', Pc.ap, Pf.ap, [Pf.k], [Pc.k])
                A, B = A2, B2
            bk = K.bank()
            for h in range(4):
                K.TR(K.pbb(bk, 128, h * 128), kT.ap[:, h, tsl], idb.ap, [(kT.k, h), idb.k], [K.pk(bk)])
            K.TT('dve', kd[x].ap, psb3(bk), bc4(be), ALU.mult, [K.pk(bk), s_.k], [kd[x].k])
            K.TT('dve', kdec[x].ap, psb3(bk), bc4(e_rev), ALU.mult, [K.pk(bk), s_.k], [kdec[x].k])
            bv = K.bank()
            for h in range(4):
                K.TR(K.pbb(bv, 128, h * 128), vT.ap[:, h, tsl], idb.ap, [(vT.k, h), idb.k], [K.pk(bv)])
            K.TT('dve', vb[x].ap, psb3(bv), bc4(beta), ALU.mult, [K.pk(bv), s_.k], [vb[x].k])
            bU = K.bank()
            for h in range(4):
                K.MM(K.pb(bU, 128, h * 128), Pc.ap[:, h, :], vb[x].ap[:, h, :], True, True, [Pc.k, vb[x].k], [K.pk(bU)])
            K.CP('act', uu[x].ap, ps3(bU), [K.pk(bU)], [uu[x].k])
            bW = K.bank()
            for h in range(4):
                K.MM(K.pb(bW, 128, h * 128), kd[x].ap[:, h, :], Pc.ap[:, h, :], True, True, [Pc.k, kd[x].k], [K.pk(bW)])
            K.CP('act', wT[x].ap, ps3(bW), [K.pk(bW)], [wT[x].k])
            bQ = K.bank()
            for h in range(4):
                K.MM(K.pb(bQ, 128, h * 128), kT.ap[:, h, tsl], qT.ap[:, h, tsl], True, True, [(kT.k, h), (qT.k, h)], [K.pk(bQ)])
            K.TT('dve', qkT[x].ap, ps3(bQ), Du[x].ap, ALU.mult, [K.pk(bQ), Du[x].k], [qkT[x].k])
            K.TT('pool', qdT[x].ap, qT.ap[:, :, tsl], eR[x].ap, ALU.mult, [(qT.k, h_) for h_ in range(4)] + [eR[x].k], [qdT[x].k])
            bz = K.bank()
            for kc in range(8):
                K.MM(K.pb(bz), F0.ap[:, kc, tsl], Wz.ap[:, kc, :], kc == 0, kc == 7, [Wz.k, (F0.k, t)], [K.pk(bz)])
            K.ACT(szt[x].ap, ps3(bz), AF.Silu, [K.pk(bz)], [szt[x].k])
            bN = K.bank()
            for h in range(4):
                K.MM(K.pb(bN, 128, h * 128), wT[x].ap[:, h, :], Sb.ap[:, h, :], True, True, [wT[x].k, Sb.k], [K.pk(bN)])
            K.TT('dve', vn[x].ap, uu[x].ap, ps3(bN), ALU.subtract, [uu[x].k, K.pk(bN)], [vn[x].k])
            bO = K.bank()
            for h in range(4):
                K.MM(K.pb(bO, 128, h * 128), qdT[x].ap[:, h, :], Sb.ap[:, h, :], True, False, [qdT[x].k, Sb.k], [K.pk(bO)])
                K.MM(K.pb(bO, 128, h * 128), qkT[x].ap[:, h, :], vn[x].ap[:, h, :], False, True, [qkT[x].k, vn[x].k], [K.pk(bO)])
            K.CP('act', of[x].ap, ps3(bO), [K.pk(bO)], [of[x].k])
            bS = K.bank()
            for h in range(4):
                K.MM(K.pb(bS, 128, h * 128), kdec[x].ap[:, h, :], vn[x].ap[:, h, :], True, True, [kdec[x].k, vn[x].k], [K.pk(bS)])
            K.TT('dve', Sf.ap, Sf.ap, bc4(e_last), ALU.mult, [Sf.k, s_.k], [Sf.k])
            K.TT('dve', Sf.ap, Sf.ap, ps3(bS), ALU.add, [Sf.k, K.pk(bS)], [Sf.k])
            K.CP('pool', Sb.ap, Sf.ap, [Sf.k], [Sb.k])
            o = of[x]
            K.ACT(sq.ap, o.ap, AF.Square, [o.k], [sq.k])
            K.s.op('dve', lambda g_: g_.tensor_reduce(out=s_.ap[:, 40:44], in_=sq.ap, axis=AX.X, op=ALU.add), [sq.k], [s_.k])
            K.TS('dve', s_.ap[:, 40:44], s_.ap[:, 40:44], 1.0 / 128.0, NORM_EPS, ALU.mult, ALU.add, [s_.k], [s_.k])
            K.ACT(s_.ap[:, 40:44], s_.ap[:, 40:44], AF.Sqrt, [s_.k], [s_.k])
            K.s.op('dve', lambda g_: g_.reciprocal(out=s_.ap[:, 44:48], in_=s_.ap[:, 40:44]), [s_.k], [s_.k])
            K.TT('dve', o.ap, o.ap, bc4(s_.ap[:, 44:48]), ALU.mult, [o.k, s_.k], [o.k])
            K.TT('pool', o.ap, o.ap, bch(nw.ap), ALU.mult, [o.k, nw.k], [o.k])
            K.TT('dve', ybf[x].ap, o.ap, szt[x].ap, ALU.mult, [o.k, szt[x].k], [ybf[x].k])
            b = K.bank()
            for h in range(4):
                K.TR(K.pbb(b, 128, h * 128), ybf[x].ap[:, h, :], idb.ap, [ybf[x].k, idb.k], [K.pk(b)])
            K.CP('act', YT.ap[:, :, tsl], psb3(b), [K.pk(b)], [(YT.k, c, t // 4) for c in range(4)])

    def merge_branch(self, l, i, F0, YT, F1):
        K = self
        d = self.d
        Wg = K.load_w(d['w_in'][l][:, C_GATE + i * 1024:C_GATE + (i + 1) * 1024], 8, 1024, 'Wg')
        Wb = K.load_w(d['w_branch'][l, i], 4, 1024, 'Wb')
        sg = [K.alloc([128, 512], F32, 'sg') for _ in range(2)]
        tm = [K.alloc([128, 512], F32, 'tm') for _ in range(2)]
        n = 0
        for dc in range(8):
            for blk in range(4):
                x = n % 2
                n += 1
                bG = K.bank()
                for kc in range(8):
                    K.MM(K.pb(bG), Wg.ap[:, kc, dc * 128:(dc + 1) * 128], F0.ap[:, kc, blk * 512:(blk + 1) * 512],
                         kc == 0, kc == 7, [Wg.k] + K.fkeys(F0, blk), [K.pk(bG)])
                bP = K.bank()
                for c in range(4):
                    K.MM(K.pb(bP), Wb.ap[:, c, dc * 128:(dc + 1) * 128], YT.ap[:, c, blk * 512:(blk + 1) * 512],
                         c == 0, c == 3, [Wb.k, (YT.k, c, blk)], [K.pk(bP)])
                K.ACT(sg[x].ap, K.pb(bG), AF.Sigmoid, [K.pk(bG)], [sg[x].k])
                dst = F1.ap[:, dc, blk * 512:(blk + 1) * 512]
                if i == 0:
                    K.TT('dve', dst, K.pb(bP), sg[x].ap, ALU.mult, [K.pk(bP), sg[x].k], [(F1.k, dc, blk)])
                else:
                    K.TT('dve', tm[x].ap, K.pb(bP), sg[x].ap, ALU.mult, [K.pk(bP), sg[x].k], [tm[x].k])
                    K.TT('pool', dst, dst, tm[x].ap, ALU.add, [(F1.k, dc, blk), tm[x].k], [(F1.k, dc, blk)])

    def ln1_block(self, l, F1, XA, tiles, x_src, x_key=None):
        K = self
        d = self.d
        with K.scope():
            Wo = K.load_w(d['w_out'][l], 8, 1024, 'Wo')
            g1 = K.load_row(d['ln1_g'][l], 1024, 'g1')
            b1 = K.load_row(d['ln1_b'][l], 1024, 'b1')
            xin = [K.alloc([128, 1024], F32, 'xin') for _ in range(2)]
            tmp = K.alloc([128, 16], F32, 'lntmp')
            for ti, t in enumerate(tiles):
                x = ti % 2
                blk = t // 4
                K.DMA('sp', xin[x].ap, x_src(t), [x_key(t)] if x_key else [], [xin[x].k])
                p2 = K.pair()
                for nb in range(2):
                    for dc in range(8):
                        K.MM(K.pb(p2 + nb), F1.ap[:, dc, t * 128:(t + 1) * 128], Wo.ap[:, dc, nb * 512:(nb + 1) * 512],
                             dc == 0, dc == 7, [(F1.k, dc, blk), Wo.k], [K.pk(p2 + nb)])
                xa = XA.ap[:, ti, :]
                xk = (XA.k, ti)
                for nb in range(2):
                    K.STT(xa[:, nb * 512:(nb + 1) * 512], xin[x].ap[:, nb * 512:(nb + 1) * 512], ALPHA, K.pb(p2 + nb),
                          ALU.mult, ALU.add, [xin[x].k, K.pk(p2 + nb)], [xk])
                K.layer_norm(xa, xk, xa, xk, g1, b1, tmp)

    def build_memT(self, s, memT):
        K = self
        with K.scope():
            mt = [K.alloc([128, 1024], F32, 'memtile') for _ in range(2)]
            for i in range(2):
                K.DMA('sp', mt[i].ap, K.d['mem'][s, i * 128:(i + 1) * 128, :], [], [mt[i].k])
                K.to_fm(mt[i].ap, mt[i].k, memT, i)

    def build_F_from_dram(self, src_tile, F):
        K = self
        with K.scope():
            xt = [K.alloc([128, 1024], F32, 'xt') for _ in range(2)]
            for t in range(SEQ // 128):
                K.DMA('sp', xt[t % 2].ap, src_tile(t), [], [xt[t % 2].k])
                K.to_fm(xt[t % 2].ap, xt[t % 2].k, F, t)

    BRANCHES = ('gdn', 'diff', 'ssd', 'mem')

    def mixer(self, l, F0, F1, memT, branches=(0, 1, 2, 3), dump=None):
        K = self
        with K.scope():
            YT = K.alloc([128, 4, SEQ], BF16, 'YT')
            first = True
            for i in branches:
                with K.scope():
                    getattr(K, 'br_' + K.BRANCHES[i])(l, F0, YT, memT)
                if dump is not None:
                    with K.scope():
                        yf = K.alloc([128, 4, SEQ], F32, 'yf')
                        K.CP('dve', yf.ap, YT.ap, [(YT.k, c, b) for c in range(4) for b in range(4)], [yf.k])
                        K.DMA('sp', dump[i].rearrange("(c p) t -> p c t", p=128), yf.ap, [yf.k], [('dump', i)])
                with K.scope():
                    K.merge_branch_i(l, i, first, F0, YT, F1)
                first = False

    def merge_branch_i(self, l, i, first, F0, YT, F1):
        save = self._merge_first = first
        K = self
        d = self.d
        Wg = K.load_w(d['w_in'][l][:, C_GATE + i * 1024:C_GATE + (i + 1) * 1024], 8, 1024, 'Wg')
        Wb = K.load_w(d['w_branch'][l, i], 4, 1024, 'Wb')
        sg = [K.alloc([128, 512], F32, 'sg') for _ in range(2)]
        tm = [K.alloc([128, 512], F32, 'tm') for _ in range(2)]
        n = 0
        for dc in range(8):
            for blk in range(4):
                x = n % 2
                n += 1
                bG = K.bank()
                for kc in range(8):
                    K.MM(K.pb(bG), Wg.ap[:, kc, dc * 128:(dc + 1) * 128], F0.ap[:, kc, blk * 512:(blk + 1) * 512],
                         kc == 0, kc == 7, [Wg.k] + K.fkeys(F0, blk), [K.pk(bG)])
                bP = K.bank()
                for c in range(4):
                    K.MM(K.pb(bP), Wb.ap[:, c, dc * 128:(dc + 1) * 128], YT.ap[:, c, blk * 512:(blk + 1) * 512],
                         c == 0, c == 3, [Wb.k, (YT.k, c, blk)], [K.pk(bP)])
                K.ACT(sg[x].ap, K.pb(bG), AF.Sigmoid, [K.pk(bG)], [sg[x].k])
                dst = F1.ap[:, dc, blk * 512:(blk + 1) * 512]
                if first:
                    K.TT('dve', dst, K.pb(bP), sg[x].ap, ALU.mult, [K.pk(bP), sg[x].k], [(F1.k, dc, blk)])
                else:
                    K.TT('dve', tm[x].ap, K.pb(bP), sg[x].ap, ALU.mult, [K.pk(bP), sg[x].k], [tm[x].k])
                    K.TT('pool', dst, dst, tm[x].ap, ALU.add, [(F1.k, dc, blk), tm[x].k], [(F1.k, dc, blk)])


def build_mixer_test(branches=(3,), dbg=None):
    K = Kern(1, [0], moe_experts=1)
    K.dbg = dbg or {}
    nc = K.nc
    K.d['dump'] = nc.dram_tensor("dump", [4, 512, SEQ], F32, kind="ExternalOutput").ap()
    K.load_consts()
    F0 = K.alloc([128, 8, SEQ], BF16, 'F0')
    F1 = K.alloc([128, 8, SEQ], BF16, 'F1')
    memT = K.alloc([128, 8, 256], BF16, 'memT')
    K.build_memT(0, memT)
    K.build_F_from_dram(lambda t: K.d['x'][0, t * 128:(t + 1) * 128, :], F0)
    K.mixer(0, F0, F1, memT, branches=branches, dump=K.d['dump'])
    with K.scope():
        XA = K.alloc([128, 8, 1024], F32, 'XA')
        for hb in range(2):
            tiles = list(range(hb * 8, hb * 8 + 8))
            K.ln1_block(0, F1, XA, tiles, lambda t: K.d['x'][0, t * 128:(t + 1) * 128, :])
            for ti, t in enumerate(tiles):
                K.DMA('sp', K.d['y'][0, t * 128:(t + 1) * 128, :], XA.ap[:, ti, :], [(XA.k, ti)], [('y', t)])
    K.s.finish()
    return K


def build_full(nseq, nlayers, experts=N_EXPERTS, loop=True):
    K = Kern(nseq, list(range(nlayers)), moe_experts=experts)
    d = K.d
    K.load_consts()
    F0 = K.alloc([128, 8, SEQ], BF16, 'F0')
    F1 = K.alloc([128, 8, SEQ], BF16, 'F1')
    memT = K.alloc([128, 8, 256], BF16, 'memT')

    def one_seq(s):
        K.build_memT(s, memT)
        K.build_F_from_dram(lambda t: d['x'][s, t * 128:(t + 1) * 128, :], F0)
        for l in range(nlayers):
            last = (l == nlayers - 1)
            K.mixer(l, F0, F1, memT)
            with K.scope():
                XA = K.alloc([128, 8, 1024], F32, 'XA')
                for hb in range(2):
                    tiles = list(range(hb * 8, hb * 8 + 8))
                    if l == 0:
                        K.ln1_block(l, F1, XA, tiles, lambda t: d['x'][s, t * 128:(t + 1) * 128, :])
                    else:
                        K.ln1_block(l, F1, XA, tiles, lambda t: d['xs'][0, t * 128:(t + 1) * 128, :], lambda t: ('x2', t))
                    if last:
                        K.moe_block(l, XA, F0, tiles, lambda t: d['y'][s, t * 128:(t + 1) * 128, :], next_F=False)
                    else:
                        K.moe_block(l, XA, F0, tiles, lambda t: d['xs'][0, t * 128:(t + 1) * 128, :], next_F=True)

    if loop:
        K.s.reset_all()
        with K.nc.Fori(0, nseq) as si:
            one_seq(si)
            K.s.reset_all()
    else:
        for s in range(nseq):
            one_seq(s)
        K.s.finish()
    return K


_CONSTS = None


def kernel(**inputs):
    global _CONSTS
    NLAUNCH = 1
    nseq = BATCH // NCORES // NLAUNCH
    K = build_full(nseq, DEPTH)
    if _CONSTS is None:
        _CONSTS = host_consts()
    x = np.ascontiguousarray(np.asarray(inputs['x'], dtype=np.float32))
    mem = np.ascontiguousarray(np.asarray(inputs['mem'], dtype=np.float32))
    shared = {n: np.ascontiguousarray(np.asarray(inputs[n], dtype=np.float32)) for n in WEIGHT_SHAPES}
    for n, v in _CONSTS.items():
        shared['c_' + n] = v
    out = np.empty((BATCH, SEQ, D_MODEL), np.float32)
    per = NCORES * nseq
    for j in range(NLAUNCH):
        in_maps = []
        for c in range(NCORES):
            m = dict(shared)
            b0 = j * per + c * nseq
            m['x'] = x[b0:b0 + nseq]
            m['mem'] = mem[b0:b0 + nseq]
            in_maps.append(m)
        res = run_bass_kernel_spmd(K.nc, in_maps, core_ids=list(range(NCORES)))
        for c in range(NCORES):
            b0 = j * per + c * nseq
            out[b0:b0 + nseq] = res.results[c]['y']
    return out


def build_moe_test(ntiles=8, experts=N_EXPERTS, dbg=None):
    K = Kern(1, [0], moe_experts=experts)
    K.dbg = dbg or {}
    K.load_consts()
    F0 = K.alloc([128, 8, SEQ], BF16, 'F0')
    XA = K.alloc([128, ntiles, 1024], F32, 'XA')
    tiles = list(range(ntiles))
    for t in tiles:
        K.DMA('sp', XA.ap[:, t, :], K.d['x'][0, t * 128:(t + 1) * 128, :], [], [(XA.k, t)])
    K.moe_block(0, XA, F0, tiles, lambda t: K.d['y'][0, t * 128:(t + 1) * 128, :], next_F=False)
    K.s.finish()
    return K
```
